# Optimizing a Trainium2 kernel written in Bass

```python
import math
import jax
import jax.numpy as jnp
from jax import lax
import numpy as np

D_MODEL = 1024
BATCH = 2
SEQ = 16384
DEPTH = 2

GRID_W = 64
CTX_LEN = 256
GLA_HEADS = 4
GLA_DK = 32
GLA_DV = 64
GLA_GATE_RANK = 16
GLA_TAU = 16.0
GLA_CHUNK = 64
HY_CH = 256
HY_ORDER = 2
HY_EMB = 33
HY_FILTER_HIDDEN = 64
HY_DECAY_TARGET = 1e-2
HY_FAST_DECAY = 0.3
HY_SLOW_DECAY = 1.5
SHORT_CONV = 3
MLA_HEADS = 8
MLA_Q_RANK = 256
MLA_KV_RANK = 128
MLA_NOPE = 64
MLA_ROPE = 32
MLA_V = 64
MLA_SCALE = (MLA_NOPE + MLA_ROPE) ** -0.5
ROPE_BASE = 10000.0
Q_BLOCK = 128
D_FF = 2816
N_EXPERTS = 8
TOP_K = 2
EXPERT_FF = 3584
D_MIX = GLA_HEADS * GLA_DV + HY_CH + MLA_HEADS * MLA_V
IN_SPLITS = (GLA_HEADS * GLA_DK, GLA_HEADS * GLA_DK, GLA_HEADS * GLA_DV, GLA_HEADS * GLA_DV,
             2 * GLA_GATE_RANK, 3 * HY_CH, MLA_Q_RANK, MLA_KV_RANK, MLA_ROPE)
IN_COLS = sum(IN_SPLITS)
F32 = jnp.float32

kernel_name = "hybrid_gla_hyena_mla_dit_block"


def _rms(x, g, eps=1e-6):
    xf = x.astype(F32)
    y = xf * lax.rsqrt(jnp.mean(xf * xf, axis=-1, keepdims=True) + eps)
    return (y * g.astype(F32)).astype(x.dtype)


def _layernorm(x, g, b, eps=1e-5):
    xf = x.astype(F32)
    mu = jnp.mean(xf, axis=-1, keepdims=True)
    var = jnp.mean(jnp.square(xf - mu), axis=-1, keepdims=True)
    return ((xf - mu) * lax.rsqrt(var + eps) * g.astype(F32) + b.astype(F32)).astype(x.dtype)


def _modulate(x, shift, scale):
    return x * (1 + scale) + shift


def _split_cols(p):
    cuts = [int(v) for v in np.cumsum(IN_SPLITS)[:-1]]
    return jnp.split(p, cuts, axis=-1)


def _heads(t, d):
    B, L, _ = t.shape
    return t.reshape(B, L, -1, d).transpose(0, 2, 1, 3).astype(F32)


def _flip(t):
    return t[:, :, ::-1]


def _gla_q(qa):
    return _heads(qa, GLA_DK) * GLA_DK ** -0.5


def _gla_kv(ka, va, alr, w_gate, b_gate):
    k = _heads(ka, GLA_DK)
    v = _heads(va, GLA_DV)
    log_a = []
    for d in range(2):
        z = alr[..., d * GLA_GATE_RANK:(d + 1) * GLA_GATE_RANK] @ w_gate[d] + b_gate[d]
        log_a.append(_heads(jax.nn.log_sigmoid(z.astype(F32)) / GLA_TAU, GLA_DK))
    return k, v, log_a[0], log_a[1]


def _gla_state(k, v, log_a):
    b = jnp.cumsum(log_a, axis=2)
    return jnp.einsum('bhtk,bhtv->bhkv', k * jnp.exp(b[:, :, -1:] - b), v)


def _gla_chunked(q, k, v, log_a, s0):
    B, H, L, DK = q.shape
    DV = v.shape[-1]
    n = L // GLA_CHUNK
    to_chunks = lambda t: jnp.moveaxis(t.reshape(B, H, n, GLA_CHUNK, t.shape[-1]), 2, 0)
    mask = jnp.tril(jnp.ones((GLA_CHUNK, GLA_CHUNK), dtype=bool))[:, :, None]

    def step(S, inp):
        qi, ki, vi, ai = inp
        b = jnp.cumsum(ai, axis=2)
        o_inter = jnp.einsum('bhtk,bhkv->bhtv', qi * jnp.exp(b), S)
        diff = b[:, :, :, None, :] - b[:, :, None, :, :]
        decay = jnp.where(mask, jnp.exp(jnp.minimum(diff, 0.0)), 0.0)
        att = jnp.einsum('bhtk,bhsk,bhtsk->bhts', qi, ki, decay)
        o = o_inter + jnp.einsum('bhts,bhsv->bhtv', att, vi)
        b_last = b[:, :, -1:, :]
        S_new = jnp.exp(b_last[:, :, 0, :])[..., None] * S + jnp.einsum(
            'bhsk,bhsv->bhkv', ki * jnp.exp(b_last - b), vi)
        return S_new, o

    _, o = lax.scan(step, s0, (to_chunks(q), to_chunks(k), to_chunks(v), to_chunks(log_a)))
    return jnp.moveaxis(o, 0, 2).reshape(B, H, L, DV)


def _gla_bidir(q, k, v, la_f, la_b, s_f, s_b):
    o_f = _gla_chunked(q, k, v, la_f, s_f)
    o_b = _flip(_gla_chunked(_flip(q), _flip(k), _flip(v), _flip(la_b), s_b))
    return o_f + o_b


def _gla_out(o, g, norm_g):
    B, H, L, DV = o.shape
    o = o.transpose(0, 2, 1, 3).astype(g.dtype)
    o = _rms(o, norm_g) * jax.nn.silu(g.reshape(B, L, H, DV))
    return o.reshape(B, L, H * DV)


def _short_conv(u, w, b):
    L = u.shape[1]
    pad = SHORT_CONV // 2
    up = jnp.pad(u, ((0, 0), (pad, pad), (0, 0)))
    y = b
    for j in range(SHORT_CONV):
        y = y + up[:, j:j + L] * w[j]
    return y


def _hyena_filters(L, fw1, fb1, ff1, fw2, fb2, ff2, fw3, fb3):
    pos = jnp.arange(L, dtype=F32)
    t = pos / (L - 1)
    bands = (HY_EMB - 1) // 2
    freqs = jnp.linspace(1e-4, bands - 1, bands, dtype=F32)
    ang = (2.0 * math.pi * pos / L)[:, None] * freqs
    z = jnp.concatenate([t[:, None], jnp.cos(ang), -jnp.sin(ang)], axis=-1)
    hid = jnp.sin(ff1.astype(F32) * (z @ fw1.astype(F32) + fb1.astype(F32)))
    hid = jnp.sin(ff2.astype(F32) * (hid @ fw2.astype(F32) + fb2.astype(F32)))
    h = (hid @ fw3.astype(F32) + fb3.astype(F32)).reshape(L, HY_ORDER, 2, HY_CH)
    deltas = jnp.abs(jnp.linspace(math.log(HY_DECAY_TARGET) / HY_SLOW_DECAY,
                                  math.log(HY_DECAY_TARGET) / HY_FAST_DECAY, HY_CH, dtype=F32))
    h = h * jnp.exp(-t[:, None, None, None] * deltas)
    return h * lax.rsqrt(jnp.sum(h * h, axis=(0, 2), keepdims=True) + 1e-6)


def _bidir_fftconv(u, h_fwd, h_bwd, d_skip):
    L = u.shape[1]
    h_circ = jnp.concatenate([h_fwd, jnp.zeros_like(h_fwd[:1]), h_bwd[:0:-1]], axis=0)
    hf = jnp.fft.rfft(h_circ, n=2 * L, axis=0)
    uf = jnp.fft.rfft(u.astype(F32), n=2 * L, axis=1)
    y = jnp.fft.irfft(uf * hf[None], n=2 * L, axis=1)[:, :L]
    return (y + u.astype(F32) * d_skip.astype(F32)).astype(u.dtype)


def _hyena(u, conv_w, conv_b, filt, skip):
    L = u.shape[1]
    u = _short_conv(u, conv_w, conv_b)
    v, x1, x2 = jnp.split(u, 3, axis=-1)
    h = _hyena_filters(L, *filt)
    z = x1 * _bidir_fftconv(v, h[:, 0, 0], h[:, 0, 1], skip[0])
    return x2 * _bidir_fftconv(z, h[:, 1, 0], h[:, 1, 1], skip[1])


def _rope_2d(x, row, col):
    half = x.shape[-1] // 2
    inv = ROPE_BASE ** (-jnp.arange(0, half, 2, dtype=F32) / half)

    def rot(xa, pos):
        ang = pos.astype(F32)[:, None, None] * inv
        cos = jnp.cos(ang).astype(x.dtype)
        sin = jnp.sin(ang).astype(x.dtype)
        x1, x2 = jnp.split(xa, 2, axis=-1)
        return jnp.concatenate([x1 * cos - x2 * sin, x1 * sin + x2 * cos], axis=-1)

    return jnp.concatenate([rot(x[..., :half], row), rot(x[..., half:], col)], axis=-1)


def _mla_q(cq, qn_g, w_uq, row, col):
    B, L, _ = cq.shape
    q = (_rms(cq, qn_g) @ w_uq).reshape(B, L, MLA_HEADS, MLA_NOPE + MLA_ROPE)
    if row is None:
        return q
    return jnp.concatenate([q[..., :MLA_NOPE], _rope_2d(q[..., MLA_NOPE:], row, col)], axis=-1)


def _mla_kv(ckv, kr, kvn_g, w_ukv, row, col):
    B, L, _ = ckv.shape
    kv = (_rms(ckv, kvn_g) @ w_ukv).reshape(B, L, MLA_HEADS, MLA_NOPE + MLA_V)
    k_nope, v = kv[..., :MLA_NOPE], kv[..., MLA_NOPE:]
    kr = kr[:, :, None, :]
    if row is not None:
        kr = _rope_2d(kr, row, col)
    k = jnp.concatenate([k_nope, jnp.broadcast_to(kr, (B, L, MLA_HEADS, MLA_ROPE))], axis=-1)
    return k, v


def _attend(q, k, v):
    s = jnp.einsum('bqhd,bkhd->bhqk', q, k).astype(F32) * MLA_SCALE
    p = jax.nn.softmax(s, axis=-1).astype(v.dtype)
    return jnp.einsum('bhqk,bkhd->bqhd', p, v)


def _attend_blocked(q, k, v):
    B, L, H, D = q.shape
    nb = L // Q_BLOCK
    qb = jnp.moveaxis(q.reshape(B, nb, Q_BLOCK, H, D), 1, 0)
    ob = lax.map(lambda t: _attend(t, k, v), qb)
    return jnp.moveaxis(ob, 0, 1).reshape(B, L, H, -1)


def _token_mixers(ph, pc, row, col, need_ctx, gla_p, hy_p, mla_p):
    B, L, _ = ph.shape
    qa, ka, va, ga, alr, hyu, cq, ckv, kr = _split_cols(ph)
    qa_c, ka_c, va_c, ga_c, alr_c, hyu_c, cq_c, ckv_c, kr_c = _split_cols(pc)
    w_gate, b_gate, gla_g = gla_p
    conv_w, conv_b, filt, skip, hy_g = hy_p
    qn_g, w_uq, kvn_g, w_ukv, mla_g = mla_p

    k_c, v_c, la_cf, la_cb = _gla_kv(ka_c, va_c, alr_c, w_gate, b_gate)
    s_f = _gla_state(k_c, v_c, la_cf)
    s_b = _gla_state(_flip(k_c), _flip(v_c), _flip(la_cb))
    km_c, vm_c = _mla_kv(ckv_c, kr_c, kvn_g, w_ukv, None, None)

    k, v, la_f, la_b = _gla_kv(ka, va, alr, w_gate, b_gate)
    y_a = _gla_out(_gla_bidir(_gla_q(qa), k, v, la_f, la_b, s_f, s_b), ga, gla_g)
    y_b = _rms(_hyena(hyu, conv_w, conv_b, filt, skip), hy_g)
    km, vm = _mla_kv(ckv, kr, kvn_g, w_ukv, row, col)
    om = _attend_blocked(_mla_q(cq, qn_g, w_uq, row, col),
                         jnp.concatenate([km_c, km], axis=1), jnp.concatenate([vm_c, vm], axis=1))
    y_c = _rms(om.reshape(B, L, -1), mla_g)
    y = jnp.concatenate([y_a, y_b, y_c], axis=-1)
    if not need_ctx:
        return y, None

    Lc = pc.shape[1]
    z0 = jnp.zeros_like(s_f)
    yc_a = _gla_out(_gla_bidir(_gla_q(qa_c), k_c, v_c, la_cf, la_cb, z0, z0), ga_c, gla_g)
    yc_b = _rms(_hyena(hyu_c, conv_w, conv_b, filt, skip), hy_g)
    omc = _attend(_mla_q(cq_c, qn_g, w_uq, None, None), km_c, vm_c)
    yc_c = _rms(omc.reshape(B, Lc, -1), mla_g)
    return y, jnp.concatenate([yc_a, yc_b, yc_c], axis=-1)


def _swiglu(h, w1, w3, w2):
    return (jax.nn.silu(h @ w1) * (h @ w3)) @ w2


def _moe(h, w_router, w1, w3, w2):
    logits = (h @ w_router).astype(F32)
    top_v, top_i = lax.top_k(logits, TOP_K)
    top_w = jax.nn.softmax(top_v, axis=-1)
    gates = jnp.sum(jax.nn.one_hot(top_i, N_EXPERTS, dtype=F32) * top_w[..., None], axis=-2).astype(h.dtype)
    out = jnp.zeros_like(h)
    for e in range(N_EXPERTS):
        out = out + gates[..., e:e + 1] * _swiglu(h, w1[e], w3[e], w2[e])
    return out


def setup_inputs(seed: int = 0) -> dict:
    key = jax.random.key(seed)
    ks = iter(jax.random.split(key, 48))
    nrm = lambda shape, scale=1.0: scale * jax.random.normal(next(ks), shape, F32)
    gain = lambda shape: 1.0 + 0.1 * jax.random.normal(next(ks), shape, F32)
    D = D_MODEL
    Hf = HY_FILTER_HIDDEN
    beta = (8.0 * DEPTH) ** -0.25
    n_dense = (DEPTH + 1) // 2
    n_moe = DEPTH // 2
    return {
        "x": nrm((BATCH, SEQ, D)),
        "c": nrm((BATCH, D)),
        "ctx": nrm((BATCH, CTX_LEN, D)),
        "c_ctx": nrm((D,)),
        "w_mod": nrm((DEPTH, D, 6 * D), 0.5 * D ** -0.5),
        "b_mod": nrm((DEPTH, 6 * D), 0.01),
        "w_in": nrm((DEPTH, D, IN_COLS), D ** -0.5),
        "gla_w_gate": nrm((DEPTH, 2, GLA_GATE_RANK, GLA_HEADS * GLA_DK), GLA_GATE_RANK ** -0.5),
        "gla_b_gate": nrm((DEPTH, 2, GLA_HEADS * GLA_DK), 0.1),
        "gla_norm_g": gain((DEPTH, GLA_DV)),
        "hy_conv_w": nrm((DEPTH, SHORT_CONV, 3 * HY_CH), SHORT_CONV ** -0.5),
        "hy_conv_b": nrm((DEPTH, 3 * HY_CH), 0.01),
        "hy_f_w1": nrm((DEPTH, HY_EMB, Hf), HY_EMB ** -0.5),
        "hy_f_b1": nrm((DEPTH, Hf), 0.1),
        "hy_f_freq1": gain((DEPTH, Hf)),
        "hy_f_w2": nrm((DEPTH, Hf, Hf), Hf ** -0.5),
        "hy_f_b2": nrm((DEPTH, Hf), 0.1),
        "hy_f_freq2": gain((DEPTH, Hf)),
        "hy_f_w3": nrm((DEPTH, Hf, HY_ORDER * 2 * HY_CH), Hf ** -0.5),
        "hy_f_b3": nrm((DEPTH, HY_ORDER * 2 * HY_CH), 0.1),
        "hy_skip": nrm((DEPTH, HY_ORDER, HY_CH)),
        "hy_norm_g": gain((DEPTH, HY_CH)),
        "mla_q_norm_g": gain((DEPTH, MLA_Q_RANK)),
        "mla_w_uq": nrm((DEPTH, MLA_Q_RANK, MLA_HEADS * (MLA_NOPE + MLA_ROPE)), MLA_Q_RANK ** -0.5),
        "mla_kv_norm_g": gain((DEPTH, MLA_KV_RANK)),
        "mla_w_ukv": nrm((DEPTH, MLA_KV_RANK, MLA_HEADS * (MLA_NOPE + MLA_V)), MLA_KV_RANK ** -0.5),
        "mla_norm_g": gain((DEPTH, MLA_HEADS * MLA_V)),
        "w_out": nrm((DEPTH, D_MIX, D), beta * D_MIX ** -0.5),
        "ln_g": gain((DEPTH, 2, D)),
        "ln_b": nrm((DEPTH, 2, D), 0.01),
        "ffn_w1": nrm((n_dense, D, D_FF), D ** -0.5),
        "ffn_w3": nrm((n_dense, D, D_FF), D ** -0.5),
        "ffn_w2": nrm((n_dense, D_FF, D), beta * D_FF ** -0.5),
        "moe_router": nrm((n_moe, D, N_EXPERTS), D ** -0.5),
        "moe_w1": nrm((n_moe, N_EXPERTS, D, EXPERT_FF), D ** -0.5),
        "moe_w3": nrm((n_moe, N_EXPERTS, D, EXPERT_FF), D ** -0.5),
        "moe_w2": nrm((n_moe, N_EXPERTS, EXPERT_FF, D), beta * EXPERT_FF ** -0.5),
    }


def reference(x, c, ctx, c_ctx, w_mod, b_mod, w_in, gla_w_gate, gla_b_gate, gla_norm_g,
              hy_conv_w, hy_conv_b, hy_f_w1, hy_f_b1, hy_f_freq1, hy_f_w2, hy_f_b2, hy_f_freq2,
              hy_f_w3, hy_f_b3, hy_skip, hy_norm_g, mla_q_norm_g, mla_w_uq, mla_kv_norm_g, mla_w_ukv,
              mla_norm_g, w_out, ln_g, ln_b, ffn_w1, ffn_w3, ffn_w2, moe_router, moe_w1, moe_w3, moe_w2):
    B, L, _ = x.shape
    ROWS = L // GRID_W
    row = jnp.broadcast_to(jnp.arange(ROWS, dtype=jnp.int32)[:, None], (ROWS, GRID_W)).reshape(-1)
    col = jnp.broadcast_to(jnp.arange(GRID_W, dtype=jnp.int32)[None, :], (ROWS, GRID_W)).reshape(-1)
    alpha = (2.0 * DEPTH) ** 0.25
    s_lat = jax.nn.silu(c)
    s_ctx = jax.nn.silu(c_ctx)
    xc = ctx
    for l in range(DEPTH):
        need_ctx = l < DEPTH - 1
        m = jnp.split((s_lat @ w_mod[l] + b_mod[l])[:, None, :], 6, axis=-1)
        mc = jnp.split(s_ctx @ w_mod[l] + b_mod[l], 6, axis=-1)
        gla_p = (gla_w_gate[l], gla_b_gate[l], gla_norm_g[l])
        filt = (hy_f_w1[l], hy_f_b1[l], hy_f_freq1[l], hy_f_w2[l], hy_f_b2[l], hy_f_freq2[l],
                hy_f_w3[l], hy_f_b3[l])
        hy_p = (hy_conv_w[l], hy_conv_b[l], filt, hy_skip[l], hy_norm_g[l])
        mla_p = (mla_q_norm_g[l], mla_w_uq[l], mla_kv_norm_g[l], mla_w_ukv[l], mla_norm_g[l])

        ph = _modulate(x, m[0], m[1]) @ w_in[l]
        pc = _modulate(xc, mc[0], mc[1]) @ w_in[l]
        y, yc = _token_mixers(ph, pc, row, col, need_ctx, gla_p, hy_p, mla_p)
        x = _layernorm(alpha * x + m[2] * (y @ w_out[l]), ln_g[l, 0], ln_b[l, 0])
        if need_ctx:
            xc = _layernorm(alpha * xc + mc[2] * (yc @ w_out[l]), ln_g[l, 0], ln_b[l, 0])

        def ffn(h, i=l // 2, dense=(l % 2 == 0)):
            if dense:
                return _swiglu(h, ffn_w1[i], ffn_w3[i], ffn_w2[i])
            return _moe(h, moe_router[i], moe_w1[i], moe_w3[i], moe_w2[i])

        x = _layernorm(alpha * x + m[5] * ffn(_modulate(x, m[3], m[4])), ln_g[l, 1], ln_b[l, 1])
        if need_ctx:
            xc = _layernorm(alpha * xc + mc[5] * ffn(_modulate(xc, mc[3], mc[4])), ln_g[l, 1], ln_b[l, 1])
    return x
```

```python
import numpy as np
import ml_dtypes
from contextlib import ExitStack

import concourse.bass as bass
import concourse.mybir as mybir
from concourse.bass_utils import run_bass_kernel_spmd

F32 = mybir.dt.float32
BF16 = mybir.dt.bfloat16
AF = mybir.ActivationFunctionType
ALU = mybir.AluOpType
AX = mybir.AxisListType

NCORES = 8
D = 1024
SEQ = 16384
BATCH = 2
CTX = 256
ENGS = ("pe", "act", "dve", "pool", "sp")
SEM_EPOCH = 30000


class Prog:
    def __init__(self):
        self.nc = bass.Bass("TRN2", target_bir_lowering=False)
        self.es = ExitStack()
        self.sem_es = ExitStack()
        self.pfx = ""
        self.ops = {e: [] for e in ENGS}
        self.nsem = 0
        self.sem = {}
        self.cnt = {}
        for e in ENGS:
            self.sem[e] = self._newsem("c_" + e)
            self.cnt[e] = 0
        self.known = {e: {} for e in ENGS}
        self.bufs = {}
        self.dsem = {}
        self.dcount = {}
        self.n_ps = 0
        self.out_events = []

    def _newsem(self, name):
        self.nsem += 1
        s = self.sem_es.enter_context(self.nc.semaphore(f"{name}_{self.nsem}"))
        return (s, f"{name}_{self.nsem}")

    def din(self, name, shape, dt=F32):
        return self.nc.dram_tensor(self.pfx + name, list(shape), dt, kind="ExternalInput").ap()

    def dout(self, name, shape, dt=F32):
        return self.nc.dram_tensor(self.pfx + name, list(shape), dt, kind="ExternalOutput").ap()

    def dscr(self, name, shape, dt=F32):
        return self.nc.dram_tensor(self.pfx + name, list(shape), dt, kind="Internal").ap()

    def sb(self, name, shape, dt=F32):
        return self.es.enter_context(self.nc.sbuf_tensor("sb_" + self.pfx + name, list(shape), dt))

    def ps(self, name, shape, dt=F32):
        return self.es.enter_context(self.nc.psum_tensor("pp_" + self.pfx + name, list(shape), dt))

    def _collect(self, eng, r, w, is_dma=False):
        need = []
        for k in r:
            st = self.bufs.get(k)
            if st and st["w"] is not None:
                need.append((st["w"], "raw"))
            if st:
                for ev in st.get("wd", {}).values():
                    need.append((ev, "raw"))
        for k in w:
            st = self.bufs.get(k)
            if st:
                if st["w"] is not None:
                    need.append((st["w"], "waw"))
                for ev in st.get("wd", {}).values():
                    need.append((ev, "waw"))
                for ev in st["r"].values():
                    need.append((ev, "war"))
        waits = {}
        for (ev, kind) in need:
            (sem, semname), val, src, srcdma = ev
            if srcdma:
                if is_dma and kind == "waw":
                    continue
                val = self.dcount[semname]
            else:
                if src == eng and not is_dma:
                    if eng == "pe":
                        continue
                    if kind != "raw":
                        continue
            if self.known[eng].get(semname, 0) >= val:
                continue
            if semname not in waits or waits[semname][1] < val:
                waits[semname] = (sem, val)
        for semname, (sem, val) in waits.items():
            self.known[eng][semname] = val
        return list(waits.values())

    def _record(self, ev, eng, r, w):
        for k in w:
            wd = {}
            if ev[3]:
                old = self.bufs.get(k)
                if old:
                    wd = dict(old.get("wd", {}))
                    if old["w"] is not None and old["w"][3]:
                        wd[old["w"][3]] = old["w"]
                wd.pop(ev[3], None)
            self.bufs[k] = {"w": ev, "r": {}, "wd": wd}
        for k in r:
            st = self.bufs.setdefault(k, {"w": None, "r": {}, "wd": {}})
            tag = ev[3] if ev[3] else eng
            st["r"][tag] = ev

    def op(self, eng, fn, r=(), w=()):
        w = list(w) + [k for k in r if isinstance(k, str) and k.startswith("ps") and k not in w]
        waits = self._collect(eng, r, w)
        if self.cnt[eng] >= SEM_EPOCH:
            self.sem[eng] = self._newsem("c_" + eng)
            self.cnt[eng] = 0
        self.cnt[eng] += 1
        sem = self.sem[eng]
        ev = (sem, self.cnt[eng], eng, None)

        def run(e, waits=waits, sem=sem[0], fn=fn):
            for (s, v) in waits:
                e.wait_ge(s, v)
            fn(e).then_inc(sem, 1)

        self.ops[eng].append(run)
        self._record(ev, eng, r, w)
        return ev

    def dma(self, eng, out, in_, sk, r=(), w=(), is_out=False):
        waits = self._collect(eng, r, w, is_dma=True)
        if sk not in self.dsem or self.dsem[sk][1] >= SEM_EPOCH:
            self.dsem[sk] = [self._newsem("d"), 0]
        ds = self.dsem[sk]
        ds[1] += 16
        self.dcount[ds[0][1]] = ds[1]
        ev = (ds[0], ds[1], eng, sk)

        def run(e, waits=waits, sem=ds[0][0], out=out, in_=in_):
            for (s, v) in waits:
                e.wait_ge(s, v)
            e.dma_start(out=out, in_=in_).then_inc(sem, 16)

        self.ops[eng].append(run)
        self._record(ev, eng, r, w)
        if is_out:
            self.out_events.append(sk)
        return ev

    def barrier(self):
        targets = [(self.sem[e], self.cnt[e]) for e in ENGS if self.cnt[e] > 0]
        for (semt, val) in [(v[0], v[1]) for v in self.dsem.values()]:
            targets.append((semt, val))
        for e in ENGS:
            waits = []
            for (sem, semname), val in targets:
                if self.known[e].get(semname, 0) >= val:
                    continue
                self.known[e][semname] = val
                waits.append((sem, val))

            def run(eng, waits=waits):
                for (s, v) in waits:
                    eng.wait_ge(s, v)

            self.ops[e].append(run)
        self.bufs = {}

    def push_scope(self):
        self._outer_es = getattr(self, "_outer_es", [])
        self._outer_es.append(self.es)
        self.es = ExitStack()

    def pop_scope(self):
        self.barrier()
        self.es.close()
        self.es = self._outer_es.pop()

    def finish(self):
        waits = []
        for sk in dict.fromkeys(self.out_events):
            (sem, semname), val = self.dsem[sk]
            waits.append((sem, val))

        def run(e, waits=waits):
            for (s, v) in waits:
                e.wait_ge(s, v)

        self.ops["sp"].append(run)
        nc = self.nc
        ops = self.ops
        with nc.Block() as block:
            @block.sync
            def _(e):
                for f in ops["sp"]:
                    f(e)

            @block.tensor
            def _(e):
                for f in ops["pe"]:
                    f(e)

            @block.scalar
            def _(e):
                for f in ops["act"]:
                    f(e)

            @block.vector
            def _(e):
                for f in ops["dve"]:
                    f(e)

            @block.gpsimd
            def _(e):
                for f in ops["pool"]:
                    f(e)
        self.es.close()
        self.sem_es.close()
        return nc


def mm(P, out, lhsT, rhs, start, stop, r, w):
    return P.op("pe", lambda e: e.matmul(out, lhsT, rhs, start=start, stop=stop), r=r, w=w)


def act(P, out, in_, func, r, w, bias=0.0, scale=1.0, accum_out=None):
    if accum_out is None:
        return P.op("act", lambda e: e.activation(out, in_, func, bias=bias, scale=scale), r=r, w=w)
    return P.op("act", lambda e: e.activation(out, in_, func, bias=bias, scale=scale,
                                              accum_out=accum_out), r=r, w=w)


def tt(P, eng, out, in0, in1, op, r, w):
    return P.op(eng, lambda e: e.tensor_tensor(out, in0, in1, op), r=r, w=w)


def ts(P, eng, out, in0, s1, s2, op0, op1, r, w):
    if s2 is None:
        return P.op(eng, lambda e: e.tensor_scalar(out, in0, s1, None, op0), r=r, w=w)
    return P.op(eng, lambda e: e.tensor_scalar(out, in0, s1, s2, op0, op1), r=r, w=w)


def stt(P, out, in0, scalar, in1, op0, op1, r, w):
    return P.op("dve", lambda e: e.scalar_tensor_tensor(out, in0, scalar, in1, op0, op1), r=r, w=w)


def cp(P, eng, out, in_, r, w):
    if eng == "act":
        return P.op("act", lambda e: e.copy(out, in_), r=r, w=w)
    return P.op(eng, lambda e: e.tensor_copy(out, in_), r=r, w=w)


def recip(P, out, in_, r, w):
    return P.op("dve", lambda e: e.reciprocal(out, in_), r=r, w=w)


class Stage:
    def __init__(self, P, name, shape, dt, n):
        self.t = [P.sb(f"{name}{i}", shape, dt) for i in range(n)]
        self.k = [f"{name}{i}" for i in range(n)]
        self.i = 0

    def get(self):
        i = self.i
        self.i = (self.i + 1) % len(self.t)
        return self.t[i], self.k[i]


class Banks:
    def __init__(self, P, n=8):
        self.t = [P.ps(f"psb{i}", [128, 512]) for i in range(n)]
        self.i = 0
        self.n = n

    def get(self):
        i = self.i
        self.i = (self.i + 1) % self.n
        return self.t[i], f"psb{i}"


RL = SEQ * BATCH // NCORES
RC = CTX * BATCH // NCORES
RR = RL + RC
NX = 2176
C_Q, C_K, C_VG, C_ALR, C_HY, C_CQ, C_CKV, C_KRP, C_KRS = 0, 128, 256, 768, 800, 1568, 1824, 1984, 2080
PERM = np.concatenate([np.arange(8, 16), np.arange(0, 8), np.arange(24, 32), np.arange(16, 24)])


def row_groups():
    g = [(512 * i, 512, 0) for i in range(RL // 512)]
    g.append((RL, RC, 1))
    return g


def emit_mod(P, B, ccT, wmod, bmodT, modT, o_mod=None):
    cs = P.sb("mod_cs", [128, 8, 2])
    sT = P.sb("mod_sT", [128, 8, 2])
    bm = P.sb("mod_bm", [128, 48])
    P.dma("sp", cs[:], ccT, ("mod_cs", "ld"), w=["mod_cs"])
    P.dma("sp", bm[:], bmodT, ("mod_bm", "ld"), w=["mod_bm"])
    act(P, sT[:], cs[:], AF.Silu, r=["mod_cs"], w=["mod_sT"])
    wv = wmod.rearrange("(k p) n -> p k n", p=128)
    wt = [P.sb(f"mod_w{i}", [128, 8, 256]) for i in range(2)]
    for cg in range(24):
        s = cg % 2
        P.dma("sp", wt[s][:], wv[:, :, cg * 256:(cg + 1) * 256], (f"mod_w{s}", "ld"), w=[f"mod_w{s}"])
        for j in range(2):
            oc = cg * 2 + j
            bank, bk = B.get()
            for k in range(8):
                mm(P, bank[:, 0:2], wt[s][:, k, j * 128:(j + 1) * 128], sT[:, k, :],
                   start=(k == 0), stop=(k == 7), r=[f"mod_w{s}", "mod_sT"], w=[bk])
            act(P, modT[:, oc, :], bank[:, 0:2], AF.Identity, r=[bk, "mod_bm"], w=["modT"],
                bias=bm[:, oc:oc + 1])
    if o_mod is not None:
        P.dma("sp", o_mod, modT[:], ("modT", "st"), r=["modT"], is_out=True)


def emit_load_xT(P, B, xd, row0, nrow, ident, xt_slots, gi, evac):
    nt = (nrow + 127) // 128
    for t in range(nt):
        rows = min(128, nrow - 128 * t)
        s = (gi * 4 + t) % len(xt_slots)
        xt = xt_slots[s]
        key = f"xt{s}"
        P.dma("sp", xt[:rows, :], xd[row0 + 128 * t: row0 + 128 * t + rows, :], (key, "ld"), w=[key])
        for half in range(2):
            bank, bk = B.get()
            for kk in range(4):
                k = half * 4 + kk
                P.op("pe", lambda e, bank=bank, kk=kk, k=k, rows=rows, xt=xt: e.transpose(
                    bank[:, kk * 128: kk * 128 + rows], xt[:rows, k * 128:(k + 1) * 128],
                    ident[:rows, :rows]), r=[key, "ident"], w=[bk])
            for kk in range(4):
                k = half * 4 + kk
                evac(k, bank[:, kk * 128: kk * 128 + rows], bk, 128 * t, rows)


def build_s1():
    P = Prog()
    emit_s1(P)
    return P.finish()


def emit_s1(P, x_fm=None):
    B = Banks(P)
    xd = P.din("x", [RR, D]) if x_fm is None else None
    ccT = P.din("ccT", [128, 8, 2])
    wmod = P.din("wmod", [D, 6 * D])
    bmodT = P.din("bmodT", [128, 48])
    win = P.din("win", [D, NX])
    identd = P.din("ident", [128, 128])
    onesd = P.din("ones", [128, 128])
    wgd = P.din("wg", [32, 2, 128])
    bgd = P.din("bg", [128, 2])
    qgd = P.din("qg", [128, 2])
    kvgd = P.din("kvg", [128, 1])
    wuqd = P.din("wuq", [256, 768])
    wuqsd = P.din("wuqs", [256, 768])
    wukvd = P.din("wukv", [128, 1024])
    ropecd = P.din("ropec", [96, RR])
    ropesd = P.din("ropes", [96, RR])

    o_mod = P.dout("o_mod", [128, 48, 2])
    o_qk = P.dout("o_qk", [2, 128, RR])
    o_la = P.dout("o_la", [2, 128, RR])
    o_vg = P.dout("o_vg", [RR, 512])
    o_hy = P.dout("o_hy", [768, RR])
    o_QT = P.dout("o_QT", [8, 96, RR], BF16)
    o_KnT = P.dout("o_KnT", [8, 64, RR], BF16)
    o_kr = P.dout("o_kr", [32, RR], BF16)
    o_V = P.dout("o_V", [RR, 512], BF16)

    ident = P.sb("ident", [128, 128])
    ones = P.sb("ones", [128, 128])
    P.dma("sp", ident[:], identd, ("ident", "ld"), w=["ident"])
    P.dma("sp", ones[:], onesd, ("ones", "ld"), w=["ones"])
    modT = P.sb("modT", [128, 48, 2])
    emit_mod(P, B, ccT, wmod, bmodT, modT, o_mod)
    sc1p = P.sb("sc1p", [128, 8, 2])
    ts(P, "dve", sc1p[:], modT[:, 8:16, :], 1.0, None, ALU.add, None, r=["modT"], w=["sc1p"])

    wx = P.sb("wx", [128, 8, NX], BF16)
    winv = win.rearrange("(k p) n -> p k n", p=128)
    for k in range(8):
        P.dma("pool", wx[:, k, :], winv[:, k, :], ("wx", "ld"), w=["wx"])
    wg = P.sb("wg", [32, 2, 128]); bg = P.sb("bg", [128, 2]); qg = P.sb("qg", [128, 2]); kvg = P.sb("kvg", [128, 1])
    P.dma("sp", wg[:], wgd, ("wg", "ld"), w=["wg"])
    P.dma("sp", bg[:], bgd, ("bg", "ld"), w=["bg"])
    P.dma("sp", qg[:], qgd, ("qg", "ld"), w=["qg"])
    P.dma("sp", kvg[:], kvgd, ("kvg", "ld"), w=["kvg"])
    wuq = P.sb("wuq", [128, 2, 768], BF16); wuqs = P.sb("wuqs", [128, 2, 768], BF16)
    wukv = P.sb("wukv", [128, 1024], BF16)
    P.dma("pool", wuq[:], wuqd.rearrange("(k p) n -> p k n", p=128), ("wuq", "ld"), w=["wuq"])
    P.dma("pool", wuqs[:], wuqsd.rearrange("(k p) n -> p k n", p=128), ("wuqs", "ld"), w=["wuqs"])
    P.dma("pool", wukv[:], wukvd, ("wukv", "ld"), w=["wukv"])
    ropecS = Stage(P, "ropec", [96, 512], F32, 2)
    ropesS = Stage(P, "ropes", [96, 512], F32, 2)

    if x_fm is None:
        xt_slots = [P.sb(f"xt{i}", [128, D]) for i in range(3)]
    else:
        S_xf = Stage(P, "xfm", [128, 8, 512], F32, 1)
    NB = 2
    hT = [P.sb(f"hT{i}", [128, 8, 512], BF16) for i in range(NB)]
    st_qk = [P.sb(f"sqk{i}", [128, 2, 512]) for i in range(NB)]
    st_la = [P.sb(f"sla{i}", [128, 2, 512]) for i in range(NB)]
    st_hy = Stage(P, "shy", [128, 512], F32, 3)
    st_vg = Stage(P, "svg", [128, 512], F32, 3)
    st_alr = [P.sb(f"salr{i}", [32, 512]) for i in range(NB)]
    st_cq = [P.sb(f"scq{i}", [128, 3, 512]) for i in range(1)] * 2
    st_sq = [P.sb(f"ssq{i}", [128, 3, 512]) for i in range(1)] * 2
    st_rs = [P.sb(f"srs{i}", [128, 2, 512]) for i in range(1)] * 2
    st_cn = [P.sb(f"scn{i}", [128, 3, 512], BF16) for i in range(NB)]
    st_QT = Stage(P, "sQT", [96, 512], BF16, 4)
    st_Kn = Stage(P, "sKn", [64, 512], BF16, 4)
    st_kr = [P.sb(f"skr{i}", [96, 512], BF16) for i in range(NB)]
    st_V = Stage(P, "sV", [128, 512], BF16, 3)
    tmpA = [P.sb(f"tmpA{i}", [96, 512]) for i in range(2)]
    tmpB = [P.sb(f"tmpB{i}", [96, 512]) for i in range(2)]
    gz = [P.sb(f"gz{i}", [128, 512]) for i in range(2)]
    ga = [P.sb(f"ga{i}", [128, 512]) for i in range(2)]
    gm = [P.sb(f"gm{i}", [128, 512]) for i in range(2)]
    tcount = [0]

    for gi, (row0, ntok, msel) in enumerate(row_groups()):
        s = gi % NB
        N = ntok
        hk = f"hT{s}"

        def evac(k, pap, bk, col0, rows, s=s, msel=msel, hk=hk):
            act(P, hT[s][:, k, col0:col0 + rows], pap, AF.Identity, r=[bk, "sc1p", "modT"], w=[hk],
                bias=modT[:, k, msel:msel + 1], scale=sc1p[:, k, msel:msel + 1])

        if x_fm is None:
            emit_load_xT(P, B, xd, row0, ntok, ident, xt_slots, gi, evac)
        else:
            xf, xfk = S_xf.get()
            P.dma("sp", xf[:, :, :N], x_fm.rearrange("(k p) n -> p k n", p=128)[:, :, row0:row0 + N], (xfk, "ld"), w=[xfk])
            for k in range(8):
                evac(k, xf[:, k, :N], xfk, 0, N)

        def fm(c0, M):
            bank, bk = B.get()
            for k in range(8):
                mm(P, bank[:M, :N], wx[:, k, c0:c0 + M], hT[s][:, k, :N], start=(k == 0), stop=(k == 7),
                   r=["wx", hk], w=[bk])
            return bank, bk

        for j, c0 in enumerate((C_Q, C_K)):
            bank, bk = fm(c0, 128)
            cp(P, "act", st_qk[s][:, j, :N], bank[:, :N], r=[bk], w=[f"sqk{s}"])
        for j in range(2):
            P.dma("sp", o_qk[j, :, row0:row0 + N], st_qk[s][:, j, :N], (f"sqk{s}", "st"), r=[f"sqk{s}"], is_out=True)
        for j in range(6):
            bank, bk = fm(C_HY + 128 * j, 128)
            stt_, sk_ = st_hy.get()
            cp(P, "dve" if j % 2 else "act", stt_[:, :N], bank[:, :N], r=[bk], w=[sk_])
            P.dma("sp", o_hy[128 * j:128 * (j + 1), row0:row0 + N], stt_[:, :N], (sk_, "st"), r=[sk_], is_out=True)
        bank, bk = fm(C_ALR, 32)
        cp(P, "dve", st_alr[s][:, :N], bank[:32, :N], r=[bk], w=[f"salr{s}"])
        for d in range(2):
            u = tcount[0] % 2
            tcount[0] += 1
            bank, bk = B.get()
            mm(P, bank[:, :N], wg[:, d, :], st_alr[s][:, :N], start=True, stop=True, r=["wg", f"salr{s}"], w=[bk])
            act(P, gz[u][:, :N], bank[:, :N], AF.Identity, r=[bk, "bg"], w=[f"gz{u}"], bias=bg[:, d:d + 1])
            act(P, ga[u][:, :N], gz[u][:, :N], AF.Abs, r=[f"gz{u}"], w=[f"ga{u}"])
            act(P, ga[u][:, :N], ga[u][:, :N], AF.Exp, r=[f"ga{u}"], w=[f"ga{u}"], scale=-1.0)
            act(P, ga[u][:, :N], ga[u][:, :N], AF.Ln, r=[f"ga{u}"], w=[f"ga{u}"], bias=1.0)
            ts(P, "dve", gm[u][:, :N], gz[u][:, :N], 0.0, 1.0 / 16.0, ALU.min, ALU.mult, r=[f"gz{u}"], w=[f"gm{u}"])
            stt(P, st_la[s][:, d, :N], ga[u][:, :N], -1.0 / 16.0, gm[u][:, :N], ALU.mult, ALU.add,
                r=[f"ga{u}", f"gm{u}"], w=[f"sla{s}"])
        for d in range(2):
            P.dma("sp", o_la[d, :, row0:row0 + N], st_la[s][:, d, :N], (f"sla{s}", "st"), r=[f"sla{s}"], is_out=True)
        nt = (N + 127) // 128
        for t in range(nt):
            rows = min(128, N - 128 * t)
            bank, bk = B.get()
            for k in range(8):
                mm(P, bank[:rows, :], hT[s][:, k, 128 * t:128 * t + rows], wx[:, k, C_VG:C_VG + 512],
                   start=(k == 0), stop=(k == 7), r=["wx", hk], w=[bk])
            stt_, sk_ = st_vg.get()
            cp(P, "dve", stt_[:rows, :], bank[:rows, :], r=[bk], w=[sk_])
            P.dma("sp", o_vg[row0 + 128 * t: row0 + 128 * t + rows, :], stt_[:rows, :], (sk_, "st"),
                  r=[sk_], is_out=True)
        for j, c0 in enumerate((C_CQ, C_CQ + 128, C_CKV)):
            bank, bk = fm(c0, 128)
            cp(P, "dve", st_cq[s][:, j, :N], bank[:, :N], r=[bk], w=["scq0"])
            act(P, st_sq[s][:, j, :N], bank[:, :N], AF.Square, r=[bk], w=["ssq0"])
        for j, (chunks, nfeat) in enumerate((((0, 1), 256.0), ((2,), 128.0))):
            bank, bk = B.get()
            for i, c in enumerate(chunks):
                mm(P, bank[:, :N], ones[:], st_sq[s][:, c, :N], start=(i == 0), stop=(i == len(chunks) - 1),
                   r=["ones", "ssq0"], w=[bk])
            act(P, st_rs[s][:, j, :N], bank[:, :N], AF.Sqrt, r=[bk], w=["srs0"], bias=1e-6, scale=1.0 / nfeat)
            recip(P, st_rs[s][:, j, :N], st_rs[s][:, j, :N], r=["srs0"], w=["srs0"])
        for c in range(3):
            j = 0 if c < 2 else 1
            gsc = qg[:, c:c + 1] if c < 2 else kvg[:, 0:1]
            stt(P, st_cn[s][:, c, :N], st_cq[s][:, c, :N], gsc, st_rs[s][:, j, :N], ALU.mult, ALU.mult,
                r=["scq0", "srs0", "qg", "kvg"], w=[f"scn{s}"])
        cs = slice(row0, row0 + N)
        ropec, rck = ropecS.get()
        ropes, rsk = ropesS.get()
        P.dma("sp", ropec[:, :N], ropecd[:, cs], (rck, "ld"), w=[rck])
        P.dma("sp", ropes[:, :N], ropesd[:, cs], (rsk, "ld"), w=[rsk])
        for h in range(8):
            b1, k1 = B.get()
            b2, k2 = B.get()
            for kk in range(2):
                mm(P, b1[:96, :N], wuq[:, kk, h * 96:(h + 1) * 96], st_cn[s][:, kk, :N], start=(kk == 0), stop=(kk == 1),
                   r=["wuq", f"scn{s}"], w=[k1])
            for kk in range(2):
                mm(P, b2[:96, :N], wuqs[:, kk, h * 96:(h + 1) * 96], st_cn[s][:, kk, :N], start=(kk == 0), stop=(kk == 1),
                   r=["wuqs", f"scn{s}"], w=[k2])
            u = tcount[0] % 2
            tcount[0] += 1
            sq_, sqk_ = st_QT.get()
            cp(P, "act", sq_[0:64, :N], b1[0:64, :N], r=[k1], w=[sqk_])
            tt(P, "dve", tmpA[u][64:96, :N], b1[64:96, :N], ropec[64:96, :N], ALU.mult, r=[k1, rck], w=[f"tmpA{u}"])
            tt(P, "dve", tmpB[u][64:96, :N], b2[64:96, :N], ropes[64:96, :N], ALU.mult, r=[k2, rsk], w=[f"tmpB{u}"])
            tt(P, "pool", sq_[64:96, :N], tmpA[u][64:96, :N], tmpB[u][64:96, :N], ALU.add,
               r=[f"tmpA{u}", f"tmpB{u}"], w=[sqk_])
            P.dma("sp", o_QT[h, :, cs], sq_[:, :N], (sqk_, "st"), r=[sqk_], is_out=True)
        for h in range(8):
            bank, bk = B.get()
            mm(P, bank[:64, :N], wukv[:, h * 128:h * 128 + 64], st_cn[s][:, 2, :N], start=True, stop=True,
               r=["wukv", f"scn{s}"], w=[bk])
            sk2_, skk_ = st_Kn.get()
            cp(P, "act", sk2_[:, :N], bank[:64, :N], r=[bk], w=[skk_])
            P.dma("sp", o_KnT[h, :, cs], sk2_[:, :N], (skk_, "st"), r=[skk_], is_out=True)
        b1, k1 = fm(C_KRP, 96)
        b2, k2 = fm(C_KRS, 96)
        u = tcount[0] % 2
        tcount[0] += 1
        tt(P, "dve", tmpA[u][64:96, :N], b1[64:96, :N], ropec[64:96, :N], ALU.mult, r=[k1, rck], w=[f"tmpA{u}"])
        tt(P, "dve", tmpB[u][64:96, :N], b2[64:96, :N], ropes[64:96, :N], ALU.mult, r=[k2, rsk], w=[f"tmpB{u}"])
        tt(P, "pool", st_kr[s][64:96, :N], tmpA[u][64:96, :N], tmpB[u][64:96, :N], ALU.add,
           r=[f"tmpA{u}", f"tmpB{u}"], w=[f"skr{s}"])
        P.dma("sp", o_kr[:, cs], st_kr[s][64:96, :N], (f"skr{s}", "st"), r=[f"skr{s}"], is_out=True)
        wv = wukv[:].rearrange("p (h c) -> p h c", c=128)[:, :, 64:128]
        for t in range(nt):
            rows = min(128, N - 128 * t)
            bank, bk = B.get()
            mm(P, bank[:rows, :].rearrange("p (h c) -> p h c", c=64), st_cn[s][:, 2, 128 * t:128 * t + rows], wv,
               start=True, stop=True, r=["wukv", f"scn{s}"], w=[bk])
            sv_, svk_ = st_V.get()
            cp(P, "act", sv_[:rows, :], bank[:rows, :], r=[bk], w=[svk_])
            P.dma("sp", o_V[row0 + 128 * t: row0 + 128 * t + rows, :], sv_[:rows, :], (svk_, "st"),
                  r=[svk_], is_out=True)
    return None


def _fm(v):
    v = np.asarray(v, np.float32)
    return np.ascontiguousarray(v.reshape(-1, 128).T)


def _rope_tables():
    t = np.arange(SEQ)
    row = (t // 64).astype(np.float32)
    col = (t % 64).astype(np.float32)
    inv = (np.float32(10000.0) ** (-np.arange(0, 16, 2, dtype=np.float32) / np.float32(16))).astype(np.float32)
    c = np.zeros((32, SEQ), np.float32)
    s = np.zeros((32, SEQ), np.float32)
    for i in range(32):
        pos = row if i < 16 else col
        ang = (pos * inv[i % 8]).astype(np.float32)
        c[i] = np.cos(ang)
        sg = -1.0 if (i % 16) < 8 else 1.0
        s[i] = sg * np.sin(ang)
    return c, s


_CONST = {}


def _consts():
    if not _CONST:
        _CONST["ident"] = np.eye(128, dtype=np.float32)
        _CONST["ones"] = np.ones((128, 128), np.float32)
        _CONST["rope"] = _rope_tables()
    return _CONST


def s1_inputs(l, inp, x_cur, xc_cur):
    C = _consts()
    rc, rs = C["rope"]
    w_in = inp["w_in"][l]
    win = np.zeros((D, NX), np.float32)
    win[:, :1984] = w_in
    win[:, C_KRP + 64:C_KRP + 96] = w_in[:, 1952:1984]
    win[:, C_KRS + 64:C_KRS + 96] = w_in[:, 1952:1984][:, PERM]
    wg = np.zeros((32, 2, 128), np.float32)
    for d in range(2):
        wg[16 * d:16 * d + 16, d, :] = inp["gla_w_gate"][l][d]
    bg = np.ascontiguousarray(inp["gla_b_gate"][l].T)
    wuq = inp["mla_w_uq"][l]
    wuqs = np.zeros_like(wuq)
    for h in range(8):
        wuqs[:, h * 96 + 64:h * 96 + 96] = wuq[:, h * 96 + 64:h * 96 + 96][:, PERM]
    xcf = xc_cur.reshape(BATCH * CTX, D)
    maps = []
    for c in range(NCORES):
        b, j = c // 4, c % 4
        xr = np.concatenate([x_cur[b, RL * j:RL * (j + 1)], xcf[RC * c:RC * (c + 1)]], 0)
        cc = np.stack([inp["c"][b], inp["c_ctx"]], 0)
        ccT = np.ascontiguousarray(cc.reshape(2, 8, 128).transpose(2, 1, 0))
        ropec = np.zeros((96, RR), np.float32)
        ropes = np.zeros((96, RR), np.float32)
        ropec[64:, :RL] = rc[:, RL * j:RL * (j + 1)]
        ropes[64:, :RL] = rs[:, RL * j:RL * (j + 1)]
        ropec[64:, RL:] = 1.0
        maps.append({
            "x": np.ascontiguousarray(xr, dtype=np.float32), "ccT": ccT,
            "wmod": inp["w_mod"][l], "bmodT": _fm(inp["b_mod"][l]), "win": win,
            "ident": C["ident"], "ones": C["ones"], "wg": wg, "bg": bg,
            "qg": _fm(inp["mla_q_norm_g"][l]), "kvg": _fm(inp["mla_kv_norm_g"][l]),
            "wuq": wuq, "wuqs": wuqs, "wukv": inp["mla_w_ukv"][l],
            "ropec": ropec, "ropes": ropes,
        })
    return maps


GC = 64
TA = CTX + SEQ + CTX
NCH = TA // GC
NSB = NCH // 8


def scan_add(P, out, data0, data1, r, w):
    return P.op("dve", lambda e: e.tensor_tensor_scan(out, data0, data1, 0.0, ALU.mult, ALU.add), r=r, w=w)


def build_gla():
    P = Prog()
    emit_gla(P)
    return P.finish()


def emit_gla(P):
    B = Banks(P)
    qqd = P.din("qq", [64, TA]); kkd = P.din("kk", [64, TA]); lad = P.din("la2", [64, TA])
    vvd = P.din("vv", [64, NCH, 64]); ggd = P.din("gg", [64, NCH, 64])
    gGd = P.din("gG", [64, 8, 64]); mFd = P.din("mF", [64, 8, 64]); mBd = P.din("mB", [64, 8, 64])
    smd = P.din("smask", [64, 512]); identd = P.din("ident", [128, 128])
    o_ya = P.dout("o_ya", [64, TA])

    ident = P.sb("ident", [128, 128]); gG = P.sb("gG", [64, 8, 64]); mF = P.sb("mF", [64, 8, 64]); mB = P.sb("mB", [64, 8, 64])
    smask = P.sb("smask", [64, 512])
    for t, d_, k in ((ident, identd, "ident"), (gG, gGd, "gG"), (mF, mFd, "mF"), (mB, mBd, "mB"), (smask, smd, "smask")):
        P.dma("sp", t[:], d_, (k, "ld"), w=[k])
    KV = P.sb("gKV", [64, NCH, 64]); X = P.sb("gX", [64, NCH, 64]); Dall = P.sb("gD", [64, NCH])
    octx = P.sb("octx", [64, 4, 64])
    S_la = Stage(P, "g_la", [64, 512], F32, 2); S_q = Stage(P, "g_q", [64, 512], F32, 2); S_k = Stage(P, "g_k", [64, 512], F32, 2)
    S_p = Stage(P, "g_p", [64, 512], F32, 2); S_u = Stage(P, "g_u", [64, 512], F32, 2)
    S_eq = Stage(P, "g_eq", [64, 512], F32, 2); S_ek = Stage(P, "g_ek", [64, 512], F32, 2)
    S_kt = Stage(P, "g_kt", [64, 8, 64], F32, 2); S_v = Stage(P, "g_v", [64, 8, 64], F32, 2); S_g = Stage(P, "g_g", [64, 8, 64], F32, 2)
    S_at = Stage(P, "g_at", [64, 8, 64], F32, 2); S_a2 = Stage(P, "g_a2", [64, 8, 64], F32, 2)
    S_o = Stage(P, "g_o", [64, 8, 64], F32, 2); S_o2 = Stage(P, "g_o2", [64, 8, 64], F32, 2)
    S_st = Stage(P, "g_st", [64, 8], F32, 2); S_y = Stage(P, "g_y", [64, 512], F32, 2)

    def prep(sb, need_q):
        cs = slice(512 * sb, 512 * sb + 512)
        la, lak = S_la.get(); kk, kkk = S_k.get()
        P.dma("sp", la[:], lad[:, cs], (lak, "ld"), w=[lak])
        P.dma("sp", kk[:], kkd[:, cs], (kkk, "ld"), w=[kkk])
        p, pk = S_p.get(); u, uk = S_u.get()
        scan_add(P, p[:], smask[:], la[:], r=["smask", lak], w=[pk])
        cp(P, "pool", u[0:32, :], p[0:32, :], r=[pk], w=[uk])
        tt(P, "pool", u[32:64, :], la[32:64, :], p[32:64, :], ALU.subtract, r=[lak, pk], w=[uk])
        act(P, Dall[:, 8 * sb:8 * sb + 8], p[:].rearrange("p (c t) -> p c t", t=64)[:, :, 63], AF.Exp, r=[pk], w=["gD"])
        ek, ekk = S_ek.get()
        act(P, ek[:], u[:], AF.Exp, r=[uk], w=[ekk], scale=-1.0)
        tt(P, "dve", ek[:], ek[:], kk[:], ALU.mult, r=[ekk, kkk], w=[ekk])
        qd = None; qdk = None
        if need_q:
            qq, qqk = S_q.get()
            P.dma("sp", qq[:], qqd[:, cs], (qqk, "ld"), w=[qqk])
            eq, eqk = S_eq.get()
            act(P, eq[:], u[:], AF.Exp, r=[uk], w=[eqk])
            stt(P, eq[:], eq[:], float(32 ** -0.5), qq[:], ALU.mult, ALU.mult, r=[eqk, qqk], w=[eqk])
            qd, qdk = eq, eqk
        return ek, ekk, qd, qdk

    for sb in range(NSB):
        kd, kdk, _, _ = prep(sb, False)
        v, vk = S_v.get()
        P.dma("sp", v[:], vvd[:, 8 * sb:8 * sb + 8, :], (vk, "ld"), w=[vk])
        bank, bk = B.get()
        for c in range(8):
            P.op("pe", lambda e, o=bank[:64, 64 * c:64 * c + 64], i=kd[:, 64 * c:64 * c + 64], idn=ident[:64, :64]:
                 e.transpose(o, i, idn), r=[kdk, "ident"], w=[bk])
        kt, ktk = S_kt.get()
        cp(P, "act", kt[:].rearrange("p c f -> p (c f)"), bank[:64, :], r=[bk], w=[ktk])
        bank2, bk2 = B.get()
        for c in range(8):
            mm(P, bank2[:64, 64 * c:64 * c + 64], kt[:, c, :], v[:, c, :], True, True, r=[ktk, vk], w=[bk2])
        for c in range(8):
            ch = 8 * sb + c
            act(P, KV[0:32, ch, :], bank2[0:32, 64 * c:64 * c + 64], AF.Copy, r=[bk2, "gD"], w=["gKV"],
                scale=Dall[0:32, ch:ch + 1])
            if ch >= 1:
                act(P, KV[32:64, ch, :], bank2[32:64, 64 * c:64 * c + 64], AF.Copy, r=[bk2, "gD"], w=["gKV"],
                    scale=Dall[32:64, ch - 1:ch])
    P.op("dve", lambda e: e.memset(X[:, 0, :], 0.0), w=["gX"])
    P.op("dve", lambda e: e.memset(X[:, NCH - 1, :], 0.0), w=["gX"])
    for c in range(NCH - 1):
        stt(P, X[0:32, c + 1, :], X[0:32, c, :], Dall[0:32, c:c + 1], KV[0:32, c, :], ALU.mult, ALU.add,
            r=["gX", "gD", "gKV"], w=["gX"])
    for c in range(NCH - 2, -1, -1):
        stt(P, X[32:64, c, :], X[32:64, c + 1, :], Dall[32:64, c:c + 1], KV[32:64, c + 1, :], ALU.mult, ALU.add,
            r=["gX", "gD", "gKV"], w=["gX"])
    for sb in range(NSB):
        kd, kdk, qd, qdk = prep(sb, True)
        v, vk = S_v.get(); g, gk = S_g.get()
        P.dma("sp", v[:], vvd[:, 8 * sb:8 * sb + 8, :], (vk, "ld"), w=[vk])
        P.dma("sp", g[:], ggd[:, 8 * sb:8 * sb + 8, :], (gk, "ld"), w=[gk])
        bf_, bfk = B.get(); bb_, bbk = B.get()
        for c in range(8):
            cc = slice(64 * c, 64 * c + 64)
            mm(P, bf_[:64, cc], kd[0:32, cc], qd[0:32, cc], True, True, r=[kdk, qdk], w=[bfk])
            mm(P, bb_[:64, cc], kd[32:64, cc], qd[32:64, cc], True, True, r=[kdk, qdk], w=[bbk])
        at, atk = S_at.get(); a2, a2k = S_a2.get()
        tt(P, "dve", at[:].rearrange("p c f -> p (c f)"), bf_[:64, :], mF[:].rearrange("p c f -> p (c f)"), ALU.mult,
           r=[bfk, "mF"], w=[atk])
        tt(P, "dve", a2[:].rearrange("p c f -> p (c f)"), bb_[:64, :], mB[:].rearrange("p c f -> p (c f)"), ALU.mult,
           r=[bbk, "mB"], w=[a2k])
        tt(P, "pool", at[:], at[:], a2[:], ALU.add, r=[atk, a2k], w=[atk])
        ob, obk = B.get()
        for c in range(8):
            ch = 8 * sb + c
            cc = slice(64 * c, 64 * c + 64)
            mm(P, ob[:64, cc], at[:, c, :], v[:, c, :], True, False, r=[atk, vk], w=[obk])
            mm(P, ob[:64, cc], qd[:, cc], X[:, ch, :], False, True, r=[qdk, "gX"], w=[obk])
        o, ok = S_o.get(); o2, o2k = S_o2.get(); st, stk = S_st.get()
        cp(P, "act", o[:].rearrange("p c f -> p (c f)"), ob[:64, :], r=[obk], w=[ok])
        if sb == 0:
            cp(P, "dve", octx[:], o[:, 0:4, :], r=[ok], w=["octx"])
        if sb == NSB - 1:
            tt(P, "dve", o[:, 4:8, :], o[:, 4:8, :], octx[:], ALU.add, r=[ok, "octx"], w=[ok])
        tt(P, "dve", o2[:], o[:], o[:], ALU.mult, r=[ok], w=[o2k])
        P.op("dve", lambda e, o_=st[:], i_=o2[:]: e.tensor_reduce(o_, i_, AX.X, ALU.add), r=[o2k], w=[stk])
        act(P, st[:], st[:], AF.Sqrt, r=[stk], w=[stk], bias=1e-6, scale=1.0 / 64.0)
        recip(P, st[:], st[:], r=[stk], w=[stk])
        act(P, o2[:], g[:], AF.Silu, r=[gk], w=[o2k])
        tt(P, "pool", o2[:], o2[:], gG[:], ALU.mult, r=[o2k, "gG"], w=[o2k])
        for c in range(8):
            stt(P, o[:, c, :], o[:, c, :], st[:, c:c + 1], o2[:, c, :], ALU.mult, ALU.mult, r=[ok, stk, o2k], w=[ok])
        tb, tbk = B.get()
        for c in range(8):
            P.op("pe", lambda e, o_=tb[:64, 64 * c:64 * c + 64], i_=o[:, c, :], idn=ident[:64, :64]:
                 e.transpose(o_, i_, idn), r=[ok, "ident"], w=[tbk])
        y, yk = S_y.get()
        cp(P, "act", y[:], tb[:64, :], r=[tbk], w=[yk])
        P.dma("sp", o_ya[:, 512 * sb:512 * sb + 512], y[:], (yk, "st"), r=[yk], is_out=True)
    return None


def _gather_fm(s1res, name, sub=None):
    lat, ctx = [], []
    for b in range(BATCH):
        parts = [s1res[4 * b + j][name] if sub is None else s1res[4 * b + j][name][sub] for j in range(4)]
        lat.append(np.concatenate([p[..., :RL] for p in parts], -1))
        ctx.append(np.concatenate([p[..., RL:] for p in parts], -1))
    return lat, ctx


def _gather_tm(s1res, name):
    lat, ctx = [], []
    for b in range(BATCH):
        parts = [s1res[4 * b + j][name] for j in range(4)]
        lat.append(np.concatenate([p[:RL] for p in parts], 0))
        ctx.append(np.concatenate([p[RL:] for p in parts], 0))
    return lat, ctx


def gla_inputs(s1res, inp, l):
    C = _consts()
    if "mF" not in C:
        s = np.arange(64)[:, None]; t = np.arange(64)[None, :]
        C["mF"] = np.ascontiguousarray(np.broadcast_to((s <= t).astype(np.float32)[:, None, :], (64, 8, 64)))
        C["mB"] = np.ascontiguousarray(np.broadcast_to((s >= t).astype(np.float32)[:, None, :], (64, 8, 64)))
        sm = np.ones((64, 512), np.float32); sm[:, ::64] = 0.0
        C["smask"] = sm
    ql, qc = _gather_fm(s1res, "o_qk", 0)
    kl, kc = _gather_fm(s1res, "o_qk", 1)
    lfl, lfc = _gather_fm(s1res, "o_la", 0)
    lbl, lbc = _gather_fm(s1res, "o_la", 1)
    vgl, vgc = _gather_tm(s1res, "o_vg")
    gG = np.ascontiguousarray(np.broadcast_to(inp["gla_norm_g"][l][None, None, :], (64, 8, 64)), dtype=np.float32)
    maps = []
    for c in range(NCORES):
        b, h = c // 4, c % 4
        hs = slice(32 * h, 32 * h + 32)
        arr = lambda lat, ctx: np.concatenate([ctx, lat, ctx], -1)
        q = arr(ql[b][hs], qc[b][hs]); k = arr(kl[b][hs], kc[b][hs])
        qf = q.copy(); qf[:, CTX + SEQ:] = 0.0
        qb = q.copy(); qb[:, :CTX] = 0.0
        la2 = np.concatenate([arr(lfl[b][hs], lfc[b][hs]), arr(lbl[b][hs], lbc[b][hs])], 0)
        vg = np.concatenate([vgc[b], vgl[b], vgc[b]], 0)
        tm = lambda x: np.ascontiguousarray(x.reshape(NCH, 64, 64).transpose(1, 0, 2))
        maps.append({
            "qq": np.ascontiguousarray(np.concatenate([qf, qb], 0)), "kk": np.ascontiguousarray(np.concatenate([k, k], 0)),
            "la2": np.ascontiguousarray(la2), "vv": tm(vg[:, 64 * h:64 * h + 64]), "gg": tm(vg[:, 256 + 64 * h:256 + 64 * h + 64]),
            "gG": gG, "mF": C["mF"], "mB": C["mB"], "smask": C["smask"], "ident": C["ident"],
        })
    return maps


MAGIC = 12582912.0
TWO_PI = float(2.0 * np.pi)
PI_LO = 3.1415925


def sinr(P, out, x, xk, t1, t1k, outk):
    ts(P, "dve", t1, x, 1.0 / TWO_PI, MAGIC, ALU.mult, ALU.add, r=[xk], w=[t1k])
    ts(P, "dve", t1, t1, -MAGIC, None, ALU.add, None, r=[t1k], w=[t1k])
    stt(P, t1, t1, -TWO_PI, x, ALU.mult, ALU.add, r=[t1k, xk], w=[t1k])
    ts(P, "dve", t1, t1, -PI_LO, PI_LO, ALU.max, ALU.min, r=[t1k], w=[t1k])
    act(P, out, t1, AF.Sin, r=[t1k], w=[outk])


def build_hyena(L):
    P = Prog()
    emit_hyena(P, L)
    return P.finish()


def emit_hyena(P, L):
    NB = L // 128
    CB = min(512, L)
    NCB = L // CB
    B = Banks(P, 6)
    ybanks = [P.ps("psy0", [128, 512]), P.ps("psy1", [128, 512])]
    hyd = P.din("hy", [96, 2, L]); cwd = P.din("cw", [96, 4])
    ZTd = P.din("ZT", [33, L]); ZTrd = P.din("ZTr", [33, L])
    fw1d = P.din("fw1", [33, 64]); fv1d = P.din("fv1", [64, 2]); fw2d = P.din("fw2", [64, 64]); fv2d = P.din("fv2", [64, 2])
    fw3d = P.din("fw3c", [64, 2, 64]); fb3d = P.din("fb3c", [64, 2]); dnegd = P.din("dneg", [64, 1]); e0d = P.din("e0", [33, 64])
    skd = P.din("skip", [128, 2, 32]); identd = P.din("ident", [128, 128]); antid = P.din("anti", [128, 128])
    onesd = P.din("ones", [128, 128])
    o_yb = P.dout("o_yb", [32, 2, L])
    gs = P.dscr("gs_scratch", [64, 2 * L], BF16)
    ucd = P.dscr("uc_scratch", [96, 2, L], F32)

    def ld(name, shape, src, dt=F32):
        t = P.sb(name, shape, dt)
        P.dma("sp", t[:], src, (name, "ld"), w=[name])
        return t

    cw = ld("cw", [96, 4], cwd); fw1 = ld("fw1", [33, 64], fw1d); fv1 = ld("fv1", [64, 2], fv1d)
    fw2 = ld("fw2", [64, 64], fw2d); fv2 = ld("fv2", [64, 2], fv2d); fw3 = ld("fw3", [64, 2, 64], fw3d)
    fb3 = ld("fb3", [64, 2], fb3d); dneg = ld("dneg", [64, 1], dnegd); e0 = ld("e0", [33, 64], e0d)
    skip = ld("skip", [128, 2, 32], skd); ident = ld("ident", [128, 128], identd); anti = ld("anti", [128, 128], antid)
    ones = ld("ones", [128, 128], onesd)
    fbs = P.sb("fbs", [64, 2])
    tt(P, "dve", fbs[:, 0:1], fv1[:, 0:1], fv1[:, 1:2], ALU.mult, r=["fv1"], w=["fbs"])
    tt(P, "dve", fbs[:, 1:2], fv2[:, 0:1], fv2[:, 1:2], ALU.mult, r=["fv2"], w=["fbs"])

    ssq = P.sb("ssq", [64, 2 * NCB])
    S_z = Stage(P, "f_z", [33, CB], F32, 2)
    S_a = Stage(P, "f_a", [64, CB], F32, 2); S_t = Stage(P, "f_t", [64, CB], F32, 2)
    S_h1 = Stage(P, "f_h1", [64, CB], F32, 2); S_h2 = Stage(P, "f_h2", [64, CB], F32, 2)
    S_w = Stage(P, "f_w", [64, CB], F32, 2); S_hr = Stage(P, "f_hr", [64, CB], F32, 2)
    S_hb = Stage(P, "f_hb", [64, CB], BF16, 2); S_sq = Stage(P, "f_sq", [64, CB], F32, 2)
    for d in range(2):
        Zsrc = ZTrd if d == 0 else ZTd
        for blk in range(NCB):
            cs = slice(CB * blk, CB * blk + CB)
            z, zk = S_z.get()
            P.dma("sp", z[:], Zsrc[:, cs], (zk, "ld"), w=[zk])
            b1, k1 = B.get()
            mm(P, b1[:64, :CB], fw1[:], z[:], True, True, r=["fw1", zk], w=[k1])
            a, ak = S_a.get(); t1, t1k = S_t.get(); h1, h1k = S_h1.get()
            act(P, a[:], b1[:64, :CB], AF.Identity, r=[k1, "fv1", "fbs"], w=[ak], bias=fbs[:, 0:1], scale=fv1[:, 0:1])
            sinr(P, h1[:], a[:], ak, t1[:], t1k, h1k)
            b2, k2 = B.get()
            mm(P, b2[:64, :CB], fw2[:], h1[:], True, True, r=["fw2", h1k], w=[k2])
            a, ak = S_a.get(); t1, t1k = S_t.get(); h2, h2k = S_h2.get()
            act(P, a[:], b2[:64, :CB], AF.Identity, r=[k2, "fv2", "fbs"], w=[ak], bias=fbs[:, 1:2], scale=fv2[:, 0:1])
            sinr(P, h2[:], a[:], ak, t1[:], t1k, h2k)
            b3, k3 = B.get(); b4, k4 = B.get()
            mm(P, b3[:64, :CB], fw3[:, d, :], h2[:], True, True, r=["fw3", h2k], w=[k3])
            mm(P, b4[:64, :CB], e0[:], z[:], True, True, r=["e0", zk], w=[k4])
            wn, wnk = S_w.get(); hr, hrk = S_hr.get(); hb, hbk = S_hb.get(); sq, sqk = S_sq.get()
            act(P, wn[:], b4[:64, :CB], AF.Exp, r=[k4, "dneg"], w=[wnk], scale=dneg[:, 0:1])
            stt(P, hr[:], b3[:64, :CB], fb3[:, d:d + 1], wn[:], ALU.add, ALU.mult, r=[k3, "fb3", wnk], w=[hrk])
            act(P, sq[:], hr[:], AF.Square, r=[hrk], w=[sqk, "ssq"], accum_out=ssq[:, d * NCB + blk:d * NCB + blk + 1])
            cp(P, "dve", hb[:], hr[:], r=[hrk], w=[hbk])
            if d == 0:
                P.dma("sp", gs[:, cs], hb[:], (hbk, "st"), r=[hbk], w=["gs"])
            else:
                lo = 1 if blk == 0 else 0
                P.dma("sp", gs[:, L - 1 + CB * blk + lo: L - 1 + CB * blk + CB], hb[:, lo:CB], (hbk, "st"), r=[hbk], w=["gs"])
    rn = P.sb("rn", [64, 1]); rnb = P.sb("rnb", [64, 128]); rnrep = P.sb("rnrep", [128, 64])
    P.op("dve", lambda e: e.tensor_reduce(rn[:], ssq[:], AX.X, ALU.add), r=["ssq"], w=["rn"])
    act(P, rn[:], rn[:], AF.Sqrt, r=["rn"], w=["rn"], bias=1e-6)
    recip(P, rn[:], rn[:], r=["rn"], w=["rn"])
    ts(P, "dve", rnb[:], ones[:64, :], rn[:, 0:1], None, ALU.mult, None, r=["ones", "rn"], w=["rnb"])
    bb, bbk = B.get()
    mm(P, bb[:, :64], rnb[:], ident[:64, :64], True, True, r=["rnb", "ident"], w=[bbk])
    cp(P, "act", rnrep[:], bb[:, :64], r=[bbk], w=["rnrep"])

    SCB = min(2048, L)
    S_xi = Stage(P, "c_xi", [96, SCB + 2], F32, 2); S_uc = Stage(P, "c_uc", [96, SCB], F32, 2)
    for b in range(2):
        for blk in range(L // SCB):
            c0 = blk * SCB
            xi, xik = S_xi.get(); uc, uck = S_uc.get()
            lo = max(c0 - 1, 0); hi = min(c0 + SCB + 1, L)
            if c0 == 0:
                P.op("pool", lambda e, o_=xi[:, 0:1]: e.memset(o_, 0.0), w=[xik])
            if c0 + SCB == L:
                P.op("pool", lambda e, o_=xi[:, SCB + 1:SCB + 2]: e.memset(o_, 0.0), w=[xik])
            P.dma("sp", xi[:, lo - (c0 - 1): hi - (c0 - 1)], hyd[:, b, lo:hi], (xik, "ld"), w=[xik])
            act(P, uc[:], xi[:, 1:SCB + 1], AF.Identity, r=[xik, "cw"], w=[uck], bias=cw[:, 3:4], scale=cw[:, 1:2])
            stt(P, uc[:], xi[:, 0:SCB], cw[:, 0:1], uc[:], ALU.mult, ALU.add, r=[xik, "cw", uck], w=[uck])
            stt(P, uc[:], xi[:, 2:SCB + 2], cw[:, 2:3], uc[:], ALU.mult, ALU.add, r=[xik, "cw", uck], w=[uck])
            P.dma("sp", ucd[:, b, c0:c0 + SCB], uc[:], (uck, "st"), r=[uck], w=["ucd"])

    Fv = P.sb("Fv", [NB, 32, 2, 128]); Fx1 = P.sb("Fx1", [NB, 32, 2, 128]); Fx2 = P.sb("Fx2", [NB, 32, 2, 128])
    for t, k, r0 in ((Fv, "Fv", 0), (Fx1, "Fx1", 32), (Fx2, "Fx2", 64)):
        for b in range(2):
            P.dma("sp", t[:, :, b, :], ucd[r0:r0 + 32, b, :].rearrange("r (I i) -> I r i", i=128), (k, "ld"), r=["ucd"], w=[k])

    NE = 2 * NB - 1
    EQ = 64
    NQ = (NE + EQ - 1) // EQ
    WQ = min(EQ, NE) * 128
    S_W = Stage(P, "Wq", [128, WQ + 1], BF16, 2)
    S_U = Stage(P, "Uc", [128, NB, 2], BF16, 2)
    S_Y = Stage(P, "Yp", [128, NB, 2], F32, 2)
    S_A = Stage(P, "ga", [NB, 2, 128], F32, 2)
    q0 = (NB - 1) // EQ
    qorder = [q0] + [q for q in range(NQ) if q != q0]
    cnt = 0
    for o in range(2):
        Fin, Fink = (Fv, "Fv") if o == 0 else (Fx1, "Fx1")
        Fg, Fgk = (Fx1, "Fx1") if o == 0 else (Fx2, "Fx2")
        for cl in range(32):
            row = o * 32 + cl
            ub, ubk = B.get()
            for b in range(2):
                P.op("pe", lambda e, o_=ub[:, b * NB:(b + 1) * NB], i_=Fin[:, cl, b, :], idn=ident[:NB, :NB]:
                     e.transpose(o_, i_, idn), r=[Fink, "ident"], w=[ubk])
            U, Uk = S_U.get()
            cp(P, "act", U[:].rearrange("p n b -> p b n"), ub[:, :2 * NB].rearrange("p (b n) -> p b n", b=2), r=[ubk], w=[Uk])
            U2 = U[:].rearrange("p n b -> p (n b)")
            yb = ybanks[cnt % 2]; ybk = f"psy{cnt % 2}"
            cnt += 1
            first = True
            nmm = NE
            done = 0
            for q in qorder:
                E0 = q * EQ
                nE = min(EQ, NE - E0)
                W, Wk = S_W.get()
                wlen = nE * 128
                src = bass.AP(tensor=gs.tensor, offset=row * 2 * L + E0 * 128, ap=[[1, 128], [1, wlen]])
                P.dma("sp", W[:, :wlen], src, (Wk, "ld"), r=["gs"], w=[Wk])
                Es = list(range(E0, E0 + nE))
                if first:
                    Es = [NB - 1] + [E for E in Es if E != NB - 1]
                for E in Es:
                    d = NB - 1 - E
                    J0 = max(0, -d); J1 = min(NB - 1, NB - 1 - d) + 1
                    done += 1
                    P.op("pe", lambda e, o_=yb[:, 2 * (J0 + d):2 * (J1 + d)], l_=W[:, (E - E0) * 128:(E - E0) * 128 + 128],
                         r_=U2[:, 2 * J0:2 * J1], st=first, sp=(done == nmm):
                         e.matmul(o_, l_, r_, start=st, stop=sp, skip_group_check=True), r=[Wk, Uk], w=[ybk])
                    first = False
            Y, Yk = S_Y.get()
            cp(P, "act", Y[:].rearrange("p n b -> p b n"), yb[:, :2 * NB].rearrange("p (n b) -> p b n", b=2), r=[ybk], w=[Yk])
            fb_, fbk = B.get()
            for b in range(2):
                mm(P, fb_[:NB, b * 128:(b + 1) * 128], Y[:].rearrange("p n b -> p b n")[:, b, :], anti[:], True, True,
                   r=[Yk, "anti"], w=[fbk])
            a, ak = S_A.get()
            ts(P, "dve", a[:].rearrange("p b n -> p (b n)"), fb_[:NB, :256], rnrep[:NB, row:row + 1], None, ALU.mult, None,
               r=[fbk, "rnrep"], w=[ak])
            stt(P, a[:], Fin[:, cl, :, :], skip[:NB, o, cl:cl + 1], a[:], ALU.mult, ALU.add, r=[Fink, "skip", ak], w=[ak])
            tt(P, "dve", Fg[:, cl, :, :], a[:], Fg[:, cl, :, :], ALU.mult, r=[ak, Fgk], w=[Fgk])
    for b in range(2):
        P.dma("sp", o_yb[:, b, :].rearrange("r (I i) -> I r i", i=128), Fx2[:, :, b, :], ("Fx2", "st"), r=["Fx2"], is_out=True)
    return None


def _hy_consts(L):
    C = _consts()
    key = ("hyz", L)
    if key not in C:
        pos = np.arange(L, dtype=np.float32)
        t = pos / np.float32(L - 1)
        freqs = np.linspace(1e-4, 15, 16, dtype=np.float32)
        ang = (np.float32(2.0 * np.pi) * pos / np.float32(L)).astype(np.float32)[:, None] * freqs
        z = np.concatenate([t[:, None], np.cos(ang), -np.sin(ang)], -1).astype(np.float32)
        ZT = np.ascontiguousarray(z.T)
        C[key] = (ZT, np.ascontiguousarray(ZT[:, ::-1]))
        lo = np.log(np.float32(1e-2)) / np.float32(1.5); hi = np.log(np.float32(1e-2)) / np.float32(0.3)
        C["hydelta"] = np.abs(np.linspace(lo, hi, 256, dtype=np.float32))
        e0 = np.zeros((33, 64), np.float32); e0[0] = 1.0
        C["e0"] = e0
        C["anti"] = np.ascontiguousarray(np.eye(128, dtype=np.float32)[::-1])
    return C[key]


def hyena_inputs(hy_b, inp, l, L):
    C = _consts()
    ZT, ZTr = _hy_consts(L)
    w3 = inp["hy_f_w3"][l]; b3 = inp["hy_f_b3"][l]
    maps = []
    for c in range(NCORES):
        ch = np.concatenate([part * 256 + 32 * c + np.arange(32) for part in range(3)])
        hy = np.ascontiguousarray(np.stack([hy_b[b][ch] for b in range(BATCH)], 1), dtype=np.float32)
        cw = np.ascontiguousarray(np.concatenate([inp["hy_conv_w"][l][:, ch].T, inp["hy_conv_b"][l][ch][:, None]], 1))
        fw3c = np.zeros((64, 2, 64), np.float32); fb3c = np.zeros((64, 2), np.float32)
        for o in range(2):
            for d in range(2):
                cols = (o * 2 + d) * 256 + 32 * c + np.arange(32)
                fw3c[:, d, o * 32:(o + 1) * 32] = w3[:, cols]
                fb3c[o * 32:(o + 1) * 32, d] = b3[cols]
        dl = C["hydelta"][32 * c:32 * c + 32]
        dneg = np.ascontiguousarray(-np.concatenate([dl, dl])[:, None])
        skip = np.ascontiguousarray(np.broadcast_to(inp["hy_skip"][l][:, 32 * c:32 * c + 32][None], (128, 2, 32)), dtype=np.float32)
        maps.append({
            "hy": hy, "cw": cw, "ZT": ZT, "ZTr": ZTr, "fw1": inp["hy_f_w1"][l],
            "fv1": np.ascontiguousarray(np.stack([inp["hy_f_freq1"][l], inp["hy_f_b1"][l]], 1)),
            "fw2": inp["hy_f_w2"][l],
            "fv2": np.ascontiguousarray(np.stack([inp["hy_f_freq2"][l], inp["hy_f_b2"][l]], 1)),
            "fw3c": fw3c, "fb3c": fb3c, "dneg": dneg, "e0": C["e0"], "skip": skip,
            "ident": C["ident"], "anti": C["anti"], "ones": C["ones"],
        })
    return maps


NK = CTX + SEQ
NKB = NK // 128
MLA_SCALE = float(96 ** -0.5)


def build_mla(need_ctx):
    P = Prog()
    emit_mla(P, need_ctx)
    return P.finish()


def emit_mla(P, need_ctx):
    B = Banks(P, 6)
    accs = [P.ps("psa0", [128, 512]), P.ps("psa1", [128, 512])]
    QAd = P.din("QA", [2, 97, SEQ], BF16); KAd = P.din("KA", [2, 97, NK], BF16)
    VAd = P.din("VA", [128, 2, NKB, 65], BF16)
    w1d = P.din("w1", [97, 1]); onesd = P.din("ones", [128, 128])
    o_att = P.dout("o_att", [2, 64, SEQ])
    if need_ctx:
        QCd = P.din("QC", [2, 97, CTX], BF16)
        o_attc = P.dout("o_attc", [2, 64, CTX])
    w1 = P.sb("w1", [97, 1]); ones = P.sb("ones", [128, 128])
    P.dma("sp", w1[:], w1d, ("w1", "ld"), w=["w1"])
    P.dma("sp", ones[:], onesd, ("ones", "ld"), w=["ones"])
    KA = [P.sb(f"KA{h}", [97, NK], BF16) for h in range(2)]
    VA = [P.sb(f"VA{h}", [128, NKB, 65], BF16) for h in range(2)]
    kmx = P.sb("kmx", [1, 40]); nk = P.sb("nk", [1, 2])
    S_sq = Stage(P, "m_sq", [97, 512], F32, 2); S_Q = Stage(P, "m_Q", [97, 512], BF16, 2)
    S_qn = Stage(P, "m_qn", [1, 512], F32, 2); S_P = Stage(P, "m_P", [128, 512], BF16, 4)
    S_o = Stage(P, "m_o", [65, 512], F32, 2); S_y = Stage(P, "m_y", [64, 512], F32, 2)
    nacc = [0]

    def qblock(h, Qsrc, q0, N, nkb, dst):
        Q, Qk = S_Q.get()
        P.dma("sp", Q[:, :N], Qsrc[h, :, q0:q0 + N], (Qk, "ld"), w=[Qk])
        sq, sqk = S_sq.get()
        act(P, sq[:, :N], Q[:, :N], AF.Square, r=[Qk], w=[sqk])
        b1, k1 = B.get()
        mm(P, b1[:1, :N], w1[:], sq[:, :N], True, True, r=["w1", sqk], w=[k1])
        qn, qnk = S_qn.get()
        act(P, qn[:, :N], b1[:1, :N], AF.Sqrt, r=[k1], w=[qnk])
        ts(P, "dve", Q[0:1, :N], qn[:, :N], nk[0:1, h:h + 1], None, ALU.mult, None, r=[qnk, "nk"], w=[Qk])
        acc = accs[nacc[0] % 2]; acck = f"psa{nacc[0] % 2}"
        nacc[0] += 1
        DEPTH = 3
        pend = {}

        def issue_qk(kb):
            sb_, sk_ = B.get()
            mm(P, sb_[:, :N], KA[h][:, kb * 128:(kb + 1) * 128], Q[:, :N], True, True, r=[f"KA{h}", Qk], w=[sk_])
            pend[kb] = (sb_, sk_)

        for kb in range(min(DEPTH, nkb)):
            issue_qk(kb)
        for kb in range(nkb):
            sb_, sk_ = pend.pop(kb)
            Pt, Pk = S_P.get()
            act(P, Pt[:, :N], sb_[:, :N], AF.Exp, r=[sk_], w=[Pk], scale=MLA_SCALE)
            if kb + DEPTH < nkb:
                issue_qk(kb + DEPTH)
            mm(P, acc[:65, :N], VA[h][:, kb, :], Pt[:, :N], kb == 0, kb == nkb - 1, r=[f"VA{h}", Pk], w=[acck])
        o, ok = S_o.get()
        cp(P, "dve", o[:, :N], acc[:65, :N], r=[acck], w=[ok])
        recip(P, o[64:65, :N], o[64:65, :N], r=[ok], w=[ok])
        b2, k2 = B.get()
        mm(P, b2[:64, :N], ones[64:65, 0:64], o[64:65, :N], True, True, r=["ones", ok], w=[k2])
        y, yk = S_y.get()
        tt(P, "dve", y[:, :N], o[0:64, :N], b2[:64, :N], ALU.mult, r=[ok, k2], w=[yk])
        P.dma("sp", dst[h, :, q0:q0 + N], y[:, :N], (yk, "st"), r=[yk], is_out=True)

    for h in range(2):
        for i in range(4):
            c0 = i * (NK // 4)
            P.dma("sp", KA[h][:, c0:c0 + NK // 4], KAd[h, :, c0:c0 + NK // 4], (f"KA{h}", "ld"), w=[f"KA{h}"])
        P.dma("sp", VA[h][:], VAd[:, h, :, :], (f"VA{h}", "ld"), w=[f"VA{h}"])
        nblk = (NK + 511) // 512
        for blk in range(nblk):
            c0 = blk * 512
            N = min(512, NK - c0)
            sq, sqk = S_sq.get()
            act(P, sq[:, :N], KA[h][:, c0:c0 + N], AF.Square, r=[f"KA{h}"], w=[sqk])
            b1, k1 = B.get()
            mm(P, b1[:1, :N], w1[:], sq[:, :N], True, True, r=["w1", sqk], w=[k1])
            P.op("dve", lambda e, o_=kmx[:, blk:blk + 1], i_=b1[:1, :N]: e.tensor_reduce(o_, i_, AX.X, ALU.max),
                 r=[k1], w=["kmx"])
        P.op("dve", lambda e, o_=nk[:, h:h + 1], i_=kmx[:, :nblk]: e.tensor_reduce(o_, i_, AX.X, ALU.max),
             r=["kmx"], w=["nk"])
        act(P, nk[:, h:h + 1], nk[:, h:h + 1], AF.Sqrt, r=["nk"], w=["nk"])
        ts(P, "dve", nk[:, h:h + 1], nk[:, h:h + 1], -1.0, None, ALU.mult, None, r=["nk"], w=["nk"])
        for qb in range(SEQ // 512):
            qblock(h, QAd, qb * 512, 512, NKB, o_att)
        if need_ctx:
            qblock(h, QCd, 0, CTX, CTX // 128, o_attc)
    return None


def mla_inputs(s1res, need_ctx):
    C = _consts()
    if "w1" not in C:
        w1 = np.ones((97, 1), np.float32); w1[0] = 0.0
        C["w1"] = w1
    bf = ml_dtypes.bfloat16
    maps = []
    QTl, QTc, Knl, Knc, krl, krc, Vl, Vc = [], [], [], [], [], [], [], []
    for b in range(BATCH):
        cores = [s1res[4 * b + j] for j in range(4)]
        QTl.append(np.concatenate([r["o_QT"][:, :, :RL] for r in cores], -1)); QTc.append(np.concatenate([r["o_QT"][:, :, RL:] for r in cores], -1))
        Knl.append(np.concatenate([r["o_KnT"][:, :, :RL] for r in cores], -1)); Knc.append(np.concatenate([r["o_KnT"][:, :, RL:] for r in cores], -1))
        krl.append(np.concatenate([r["o_kr"][:, :RL] for r in cores], -1)); krc.append(np.concatenate([r["o_kr"][:, RL:] for r in cores], -1))
        Vl.append(np.concatenate([r["o_V"][:RL] for r in cores], 0)); Vc.append(np.concatenate([r["o_V"][RL:] for r in cores], 0))
    for c in range(NCORES):
        b, p = c // 4, c % 4
        QA = np.zeros((2, 97, SEQ), bf); KA = np.ones((2, 97, NK), bf); VA = np.ones((128, 2, NKB, 65), bf)
        QC = np.zeros((2, 97, CTX), bf)
        for i in range(2):
            h = 2 * p + i
            QA[i, 1:] = QTl[b][h]; QC[i, 1:] = QTc[b][h]
            KA[i, 1:65, :CTX] = Knc[b][h]; KA[i, 1:65, CTX:] = Knl[b][h]
            KA[i, 65:, :CTX] = krc[b]; KA[i, 65:, CTX:] = krl[b]
            V = np.concatenate([Vc[b][:, 64 * h:64 * h + 64], Vl[b][:, 64 * h:64 * h + 64]], 0)
            VA[:, i, :, :64] = V.reshape(NKB, 128, 64).transpose(1, 0, 2)
        m = {"QA": QA, "KA": KA, "VA": VA, "w1": C["w1"], "ones": C["ones"]}
        if need_ctx:
            m["QC"] = QC
        maps.append(m)
    return maps


ALPHA = float((2.0 * 2) ** 0.25)
FB = 512


def emit_ln(P, B, ones, u, uk, N, gcol, bcol, out_fn, tmps):
    usq, usqk, st, stk = tmps
    for k in range(8):
        act(P, usq[:, k, :N], u[:, k, :N], AF.Square, r=[uk], w=[f"{usqk}_{k}"])
    b1, k1 = B.get(); b2, k2 = B.get()
    for k in range(8):
        mm(P, b1[:, :N], ones[:], u[:, k, :N], k == 0, k == 7, r=["ones", uk], w=[k1])
    for k in range(8):
        mm(P, b2[:, :N], ones[:], usq[:, k, :N], k == 0, k == 7, r=["ones", f"{usqk}_{k}"], w=[k2])
    ts(P, "dve", st[:, 0, :N], b1[:, :N], 1.0 / D, None, ALU.mult, None, r=[k1], w=[stk])
    tt(P, "dve", st[:, 2, :N], st[:, 0, :N], st[:, 0, :N], ALU.mult, r=[stk], w=[stk])
    stt(P, st[:, 1, :N], b2[:, :N], 1.0 / D, st[:, 2, :N], ALU.mult, ALU.subtract, r=[k2, stk], w=[stk])
    act(P, st[:, 1, :N], st[:, 1, :N], AF.Sqrt, r=[stk], w=[stk], bias=1e-5)
    recip(P, st[:, 1, :N], st[:, 1, :N], r=[stk], w=[stk])
    tt(P, "dve", st[:, 2, :N], st[:, 0, :N], st[:, 1, :N], ALU.mult, r=[stk], w=[stk])
    for k in range(8):
        kk = f"{usqk}_{k}"
        e1 = "pool" if k % 2 else "dve"
        tt(P, e1, usq[:, k, :N], u[:, k, :N], st[:, 1, :N], ALU.mult, r=[uk, stk], w=[kk])
        tt(P, e1, usq[:, k, :N], usq[:, k, :N], st[:, 2, :N], ALU.subtract, r=[kk, stk], w=[kk])
        out_fn(k, usq[:, k, :N], kk)


def build_s4(kind, need_ctx):
    P = Prog()
    emit_s4(P, kind, need_ctx)
    return P.finish()


def emit_s4(P, kind, need_ctx):
    NE_ = 1 if kind == "dense" else 8
    FF = 2816 if kind == "dense" else 3584
    R4 = RR if need_ctx else RL
    B = Banks(P)
    xd = P.din("x", [R4, D]); yad = P.din("ya", [256, R4]); ybd = P.din("yb", [256, R4]); ycd = P.din("yc", [512, R4])
    modd = P.din("mod", [128, 48, 2]); identd = P.din("ident", [128, 128]); onesd = P.din("ones", [128, 128])
    hygd = P.din("hyg", [128, 2]); mlagd = P.din("mlag", [128, 4]); woutd = P.din("wout", [D, D])
    lngd = P.din("lng", [128, 2, 8]); lnbd = P.din("lnb", [128, 2, 8])
    w1d = P.din("w1", [NE_, D, FF]); w3d = P.din("w3", [NE_, D, FF]); w2d = P.din("w2", [NE_, FF, D])
    if kind == "moe":
        wrd = P.din("wr", [D, 8]); seld = P.din("sel", [8, 8, 128])
    o_xT = P.dout("o_xT", [D, R4])
    x1d = P.dscr("x1_scratch", [D, R4], F32)
    hmd = P.dscr("hm_scratch", [D, R4], BF16)
    gtd = P.dscr("gt_scratch", [8, R4], F32)

    def ld(name, shape, src, dt=F32, eng="sp"):
        t = P.sb(name, shape, dt)
        P.dma(eng, t[:], src, (name, "ld"), w=[name])
        return t

    ident = ld("ident", [128, 128], identd); ones = ld("ones", [128, 128], onesd)
    modT = ld("modT", [128, 48, 2], modd); lng = ld("lng", [128, 2, 8], lngd); lnb = ld("lnb", [128, 2, 8], lnbd)
    sc2p = P.sb("sc2p", [128, 8, 2])
    ts(P, "dve", sc2p[:], modT[:, 32:40, :], 1.0, None, ALU.add, None, r=["modT"], w=["sc2p"])
    groups = [(512 * i, 512, 0) for i in range(RL // 512)] + ([(RL, RC, 1)] if need_ctx else [])

    P.push_scope()
    hyg = ld("hyg", [128, 2], hygd); mlag = ld("mlag", [128, 4], mlagd)
    wout = ld("wout", [128, 8, D], woutd.rearrange("(k p) n -> p k n", p=128), BF16, "pool")
    if kind == "moe":
        wr = ld("wr", [128, 8, 8], wrd.rearrange("(k p) n -> p k n", p=128))
    xt_slots = [P.sb(f"xt{i}", [128, D]) for i in range(2)]
    S_xa = Stage(P, "xa", [128, 8, 512], F32, 1); S_yi = Stage(P, "yi", [128, 8, 512], F32, 1)
    S_sq = Stage(P, "ysq", [128, 6, 512], F32, 1); S_rs = Stage(P, "yrs", [128, 2, 512], F32, 1)
    S_yb = Stage(P, "ybf", [128, 8, 512], BF16, 2); S_u = Stage(P, "u1", [128, 8, 512], F32, 1)
    S_usq = Stage(P, "usq", [128, 8, 512], F32, 1); S_st = Stage(P, "lst", [128, 3, 512], F32, 1)
    S_x1 = Stage(P, "x1", [128, 8, 512], F32, 1); S_hm = Stage(P, "hm", [128, 8, 512], F32, 1)
    S_hb = Stage(P, "hmb", [128, 8, 512], BF16, 2)
    S_lg = Stage(P, "lg", [128, 8], F32, 2); S_e1 = Stage(P, "e1", [128, 8], F32, 2); S_e2 = Stage(P, "e2", [128, 8], F32, 2)
    S_m = Stage(P, "mx", [128, 4], F32, 2); S_gt = Stage(P, "gT", [8, 512], F32, 2)
    for gi, (row0, N, msel) in enumerate(groups):
        cs = slice(row0, row0 + N)
        xa, xak = S_xa.get()

        def evac(k, pap, bk, col0, rows, xa=xa, xak=xak):
            act(P, xa[:, k, col0:col0 + rows], pap, AF.Copy, r=[bk], w=[xak], scale=ALPHA)

        emit_load_xT(P, B, xd, row0, N, ident, xt_slots, gi, evac)
        yi, yik = S_yi.get()
        P.dma("sp", yi[:, 0:2, :N], yad.rearrange("(k p) n -> p k n", p=128)[:, :, cs], (yik, "ld"), w=[yik])
        P.dma("sp", yi[:, 2:4, :N], ybd.rearrange("(k p) n -> p k n", p=128)[:, :, cs], (yik, "ld"), w=[yik])
        P.dma("sp", yi[:, 4:8, :N], ycd.rearrange("(k p) n -> p k n", p=128)[:, :, cs], (yik, "ld"), w=[yik])
        sq, sqk = S_sq.get(); rs, rsk = S_rs.get(); ybf, ybk = S_yb.get()
        for k in range(2, 8):
            act(P, sq[:, k - 2, :N], yi[:, k, :N], AF.Square, r=[yik], w=[sqk])
        for j, (ks, nf) in enumerate((((0, 1), 256.0), ((2, 3, 4, 5), 512.0))):
            bank, bk = B.get()
            for i, k in enumerate(ks):
                mm(P, bank[:, :N], ones[:], sq[:, k, :N], i == 0, i == len(ks) - 1, r=["ones", sqk], w=[bk])
            act(P, rs[:, j, :N], bank[:, :N], AF.Sqrt, r=[bk], w=[rsk], bias=1e-6, scale=1.0 / nf)
            recip(P, rs[:, j, :N], rs[:, j, :N], r=[rsk], w=[rsk])
        for k in range(2):
            cp(P, "pool", ybf[:, k, :N], yi[:, k, :N], r=[yik], w=[ybk])
        for k in range(2, 8):
            gs_ = hyg[:, k - 2:k - 1] if k < 4 else mlag[:, k - 4:k - 3]
            stt(P, ybf[:, k, :N], yi[:, k, :N], gs_, rs[:, 0 if k < 4 else 1, :N], ALU.mult, ALU.mult,
                r=[yik, rsk, "hyg", "mlag"], w=[ybk])
        u, uk = S_u.get()
        for dc in range(8):
            bank, bk = B.get()
            for k in range(8):
                mm(P, bank[:, :N], wout[:, k, dc * 128:(dc + 1) * 128], ybf[:, k, :N], k == 0, k == 7, r=["wout", ybk], w=[bk])
            stt(P, u[:, dc, :N], bank[:, :N], modT[:, 16 + dc, msel:msel + 1], xa[:, dc, :N], ALU.mult, ALU.add,
                r=[bk, "modT", xak], w=[uk])
        usq, usqk = S_usq.get(); st, stk = S_st.get(); x1, x1k = S_x1.get(); hm, hmk = S_hm.get(); hb, hbk = S_hb.get()

        def out1(k, xn, xnk, x1=x1, x1k=x1k, hm=hm, hmk=hmk, hb=hb, hbk=hbk, N=N, msel=msel):
            act(P, x1[:, k, :N], xn, AF.Identity, r=[xnk, "lng", "lnb"], w=[x1k], bias=lnb[:, 0, k:k + 1], scale=lng[:, 0, k:k + 1])
            act(P, hm[:, k, :N], x1[:, k, :N], AF.Identity, r=[x1k, "sc2p", "modT"], w=[hmk],
                bias=modT[:, 24 + k, msel:msel + 1], scale=sc2p[:, k, msel:msel + 1])
            cp(P, "pool", hb[:, k, :N], hm[:, k, :N], r=[hmk], w=[hbk])

        emit_ln(P, B, ones, u, uk, N, None, None, out1, (usq, usqk, st, stk))
        P.dma("sp", x1d.rearrange("(k p) n -> p k n", p=128)[:, :, cs], x1[:, :, :N], (x1k, "st"), r=[x1k], w=["x1d"])
        P.dma("sp", hmd.rearrange("(k p) n -> p k n", p=128)[:, :, cs], hb[:, :, :N], (hbk, "st"), r=[hbk], w=["hmd"])
        if kind == "moe":
            gT, gTk = S_gt.get()
            gb, gbk = B.get()
            for t in range((N + 127) // 128):
                rows = min(128, N - 128 * t)
                bank, bk = B.get()
                for k in range(8):
                    mm(P, bank[:rows, 0:8], hm[:, k, 128 * t:128 * t + rows], wr[:, k, :], k == 0, k == 7, r=[hmk, "wr"], w=[bk])
                lg, lgk = S_lg.get(); e1, e1k = S_e1.get(); e2, e2k = S_e2.get(); mx, mxk = S_m.get()
                cp(P, "dve", lg[:rows, :], bank[:rows, 0:8], r=[bk], w=[lgk])
                P.op("dve", lambda e, o_=mx[:rows, 0:1], i_=lg[:rows, :]: e.tensor_reduce(o_, i_, AX.X, ALU.max), r=[lgk], w=[mxk])
                ts(P, "dve", e1[:rows, :], lg[:rows, :], mx[:rows, 0:1], None, ALU.is_equal, None, r=[lgk, mxk], w=[e1k])
                stt(P, e2[:rows, :], e1[:rows, :], -1e30, lg[:rows, :], ALU.mult, ALU.add, r=[e1k, lgk], w=[e2k])
                P.op("dve", lambda e, o_=mx[:rows, 1:2], i_=e2[:rows, :]: e.tensor_reduce(o_, i_, AX.X, ALU.max), r=[e2k], w=[mxk])
                ts(P, "dve", e2[:rows, :], e2[:rows, :], mx[:rows, 1:2], None, ALU.is_equal, None, r=[e2k, mxk], w=[e2k])
                tt(P, "dve", mx[:rows, 2:3], mx[:rows, 0:1], mx[:rows, 1:2], ALU.subtract, r=[mxk], w=[mxk])
                act(P, mx[:rows, 2:3], mx[:rows, 2:3], AF.Sigmoid, r=[mxk], w=[mxk])
                ts(P, "dve", mx[:rows, 3:4], mx[:rows, 2:3], -1.0, 1.0, ALU.mult, ALU.add, r=[mxk], w=[mxk])
                ts(P, "dve", e1[:rows, :], e1[:rows, :], mx[:rows, 2:3], None, ALU.mult, None, r=[e1k, mxk], w=[e1k])
                stt(P, e1[:rows, :], e2[:rows, :], mx[:rows, 3:4], e1[:rows, :], ALU.mult, ALU.add, r=[e2k, mxk, e1k], w=[e1k])
                P.op("pe", lambda e, o_=gb[:8, 128 * t:128 * t + rows], i_=e1[:rows, :], idn=ident[:rows, :rows]:
                     e.transpose(o_, i_, idn), r=[e1k, "ident"], w=[gbk])
            cp(P, "act", gT[:, :N], gb[:8, :N], r=[gbk], w=[gTk])
            P.dma("sp", gtd[:, cs], gT[:, :N], (gTk, "st"), r=[gTk], w=["gtd"])
    P.pop_scope()

    spans = [(0, 2048), (2048, 2048)] + ([(RL, RC)] if need_ctx else [])
    hmS = P.sb("hmS", [128, 8, 2048], BF16); acc = P.sb("accS", [128, 8, 2048], F32)
    if kind == "moe":
        sel = ld("sel", [8, 8, 128], seld)
        gts = P.sb("gts", [8, 2048]); Gb = P.sb("Gb", [128, 4, 512], F32)
    S_w1 = Stage(P, "fw1", [128, 8, FB], BF16, 2); S_w3 = Stage(P, "fw3", [128, 8, FB], BF16, 2)
    S_w2 = Stage(P, "fw2", [128, 4, D], BF16, 2)
    S_s = Stage(P, "fs", [128, 512], F32, 2); S_t = Stage(P, "ft", [128, 512], F32, 2)
    S_g = Stage(P, "fg", [128, 4, 512], BF16, 2)
    S_x1b = Stage(P, "x1b", [128, 8, 128], F32, 1)
    S_usq2 = Stage(P, "usq2", [128, 8, 128], F32, 1); S_st2 = Stage(P, "lst2", [128, 3, 128], F32, 1)
    S_out = Stage(P, "xo", [128, 8, 128], F32, 1)
    nfb = [(f0, min(FB, FF - f0)) for f0 in range(0, FF, FB)]
    for (t0, TN) in spans:
        msel = 1 if t0 >= RL else 0
        P.dma("sp", hmS[:, :, :TN], hmd.rearrange("(k p) n -> p k n", p=128)[:, :, t0:t0 + TN], ("hmS", "ld"), r=["hmd"], w=["hmS"])
        if kind == "moe":
            P.dma("sp", gts[:, :TN], gtd[:, t0:t0 + TN], ("gts", "ld"), r=["gtd"], w=["gts"])
        sub = [(g0, min(512, TN - g0)) for g0 in range(0, TN, 512)]
        pendB = []

        def phase_b(item):
            (w2t, w2k, nfc, g, gk, g0, N, first) = item
            for dc in range(8):
                bank, bk = B.get()
                for fc in range(nfc):
                    mm(P, bank[:, :N], w2t[:, fc, dc * 128:(dc + 1) * 128], g[:, fc, :N], fc == 0, fc == nfc - 1, r=[w2k, gk], w=[bk])
                if first:
                    cp(P, "dve", acc[:, dc, g0:g0 + N], bank[:, :N], r=[bk], w=["accS"])
                else:
                    tt(P, "dve", acc[:, dc, g0:g0 + N], acc[:, dc, g0:g0 + N], bank[:, :N], ALU.add, r=[bk, "accS"], w=["accS"])

        first_acc = True
        for e_ in range(NE_):
            if kind == "moe":
                for gi, (g0, N) in enumerate(sub):
                    bank, bk = B.get()
                    mm(P, bank[:, :N], sel[:, e_, :], gts[:, g0:g0 + N], True, True, r=["sel", "gts"], w=[bk])
                    cp(P, "act", Gb[:, gi, :N], bank[:, :N], r=[bk], w=["Gb"])
            for (f0, fn) in nfb:
                nfc = fn // 128
                w1t, w1k = S_w1.get(); w3t, w3k = S_w3.get(); w2t, w2k = S_w2.get()
                P.dma("pool", w1t[:, :, :fn], w1d[e_].rearrange("(k p) n -> p k n", p=128)[:, :, f0:f0 + fn], (w1k, "ld"), w=[w1k])
                P.dma("pool", w3t[:, :, :fn], w3d[e_].rearrange("(k p) n -> p k n", p=128)[:, :, f0:f0 + fn], (w3k, "ld"), w=[w3k])
                P.dma("pool", w2t[:, :nfc, :], w2d[e_, f0:f0 + fn, :].rearrange("(k p) n -> p k n", p=128), (w2k, "ld"), w=[w2k])
                for gi, (g0, N) in enumerate(sub):
                    g, gk = S_g.get()
                    for fc in range(nfc):
                        b1, k1 = B.get(); b3, k3 = B.get()
                        for k in range(8):
                            mm(P, b1[:, :N], w1t[:, k, fc * 128:(fc + 1) * 128], hmS[:, k, g0:g0 + N], k == 0, k == 7, r=[w1k, "hmS"], w=[k1])
                        for k in range(8):
                            mm(P, b3[:, :N], w3t[:, k, fc * 128:(fc + 1) * 128], hmS[:, k, g0:g0 + N], k == 0, k == 7, r=[w3k, "hmS"], w=[k3])
                        s_, sk = S_s.get()
                        act(P, s_[:, :N], b1[:, :N], AF.Silu, r=[k1], w=[sk])
                        if kind == "moe":
                            t_, tk = S_t.get()
                            tt(P, "dve", t_[:, :N], s_[:, :N], b3[:, :N], ALU.mult, r=[sk, k3], w=[tk])
                            tt(P, "pool", g[:, fc, :N], t_[:, :N], Gb[:, gi, :N], ALU.mult, r=[tk, "Gb"], w=[gk])
                        else:
                            tt(P, "dve", g[:, fc, :N], s_[:, :N], b3[:, :N], ALU.mult, r=[sk, k3], w=[gk])
                    if pendB:
                        phase_b(pendB.pop())
                    pendB.append((w2t, w2k, nfc, g, gk, g0, N, first_acc))
                first_acc = False
        if pendB:
            phase_b(pendB.pop())
        for (g0, N) in [(g0, min(128, TN - g0)) for g0 in range(0, TN, 128)]:
            cs = slice(t0 + g0, t0 + g0 + N)
            x1b, x1bk = S_x1b.get(); u2, u2k = x1b, x1bk
            P.dma("sp", x1b[:, :, :N], x1d.rearrange("(k p) n -> p k n", p=128)[:, :, cs], (x1bk, "ld"), r=["x1d"], w=[x1bk])
            for k in range(8):
                ts(P, "pool", x1b[:, k, :N], x1b[:, k, :N], ALPHA, None, ALU.mult, None, r=[x1bk], w=[x1bk])
                stt(P, u2[:, k, :N], acc[:, k, g0:g0 + N], modT[:, 40 + k, msel:msel + 1], x1b[:, k, :N], ALU.mult, ALU.add,
                    r=["accS", "modT", x1bk], w=[u2k])
            usq, usqk = S_usq2.get(); st, stk = S_st2.get(); xo, xok = S_out.get()

            def out2(k, xn, xnk, xo=xo, xok=xok, N=N):
                act(P, xo[:, k, :N], xn, AF.Identity, r=[xnk, "lng", "lnb"], w=[xok], bias=lnb[:, 1, k:k + 1], scale=lng[:, 1, k:k + 1])

            emit_ln(P, B, ones, u2, u2k, N, None, None, out2, (usq, usqk, st, stk))
            P.dma("sp", o_xT.rearrange("(k p) n -> p k n", p=128)[:, :, cs], xo[:, :, :N], (xok, "st"), r=[xok], is_out=True)
    return o_xT


def s4_inputs(l, inp, x_cur, xc_cur, s1res, glares, hyres, hycres, mlares, need_ctx):
    C = _consts()
    if "sel" not in C:
        sel = np.zeros((8, 8, 128), np.float32)
        for e in range(8):
            sel[e, e, :] = 1.0
        C["sel"] = sel
    R4 = RR if need_ctx else RL
    ya_l = [np.concatenate([glares[4 * b + h]["o_ya"][:, CTX:CTX + SEQ] for h in range(4)], 0) for b in range(BATCH)]
    ya_c = [np.concatenate([glares[4 * b + h]["o_ya"][:, CTX + SEQ:] for h in range(4)], 0) for b in range(BATCH)]
    yb_l = [np.concatenate([hyres[c]["o_yb"][:, b, :] for c in range(NCORES)], 0) for b in range(BATCH)]
    yc_l = [np.concatenate([mlares[4 * b + p]["o_att"].reshape(128, SEQ) for p in range(4)], 0) for b in range(BATCH)]
    if need_ctx:
        yb_c = [np.concatenate([hycres[c]["o_yb"][:, b, :] for c in range(NCORES)], 0) for b in range(BATCH)]
        yc_c = [np.concatenate([mlares[4 * b + p]["o_attc"].reshape(128, CTX) for p in range(4)], 0) for b in range(BATCH)]
    xcf = xc_cur.reshape(BATCH * CTX, D)
    dense = (l % 2 == 0)
    i = l // 2
    maps = []
    for c in range(NCORES):
        b, j = c // 4, c % 4
        ls = slice(RL * j, RL * (j + 1))
        cs = slice(RC * j, RC * (j + 1))
        def cat(lat, ctx):
            if need_ctx:
                return np.ascontiguousarray(np.concatenate([lat[b][:, ls], ctx[b][:, cs]], 1), dtype=np.float32)
            return np.ascontiguousarray(lat[b][:, ls], dtype=np.float32)
        xr = x_cur[b, ls]
        if need_ctx:
            xr = np.concatenate([xr, xcf[RC * c:RC * (c + 1)]], 0)
        m = {
            "x": np.ascontiguousarray(xr, dtype=np.float32), "ya": cat(ya_l, ya_c),
            "yb": cat(yb_l, yb_c if need_ctx else None), "yc": cat(yc_l, yc_c if need_ctx else None),
            "mod": s1res[c]["o_mod"], "ident": C["ident"], "ones": C["ones"],
            "hyg": _fm(inp["hy_norm_g"][l]), "mlag": _fm(inp["mla_norm_g"][l]), "wout": inp["w_out"][l],
            "lng": np.ascontiguousarray(inp["ln_g"][l].reshape(2, 8, 128).transpose(2, 0, 1)),
            "lnb": np.ascontiguousarray(inp["ln_b"][l].reshape(2, 8, 128).transpose(2, 0, 1)),
        }
        if dense:
            m["w1"] = inp["ffn_w1"][i][None]; m["w3"] = inp["ffn_w3"][i][None]; m["w2"] = inp["ffn_w2"][i][None]
        else:
            m["w1"] = inp["moe_w1"][i]; m["w3"] = inp["moe_w3"][i]; m["w2"] = inp["moe_w2"][i]
            m["wr"] = inp["moe_router"][i]; m["sel"] = C["sel"]
        maps.append(m)
    return maps


_PROGS = {}


def _prog(name, fn, *args):
    key = (name,) + args
    if key not in _PROGS:
        _PROGS[key] = fn(*args)
    return _PROGS[key]


def _run(nc, maps):
    return run_bass_kernel_spmd(nc, maps, core_ids=list(range(NCORES))).results


def _scoped(P, pfx, fn, *args):
    P.push_scope()
    P.pfx = pfx
    r = fn(P, *args)
    P.pop_scope()
    P.pfx = ""
    return r


def build_mix(need_ctx):
    P = Prog()
    _scoped(P, "g_", emit_gla)
    _scoped(P, "h_", emit_hyena, SEQ)
    if need_ctx:
        _scoped(P, "c_", emit_hyena, CTX)
    _scoped(P, "m_", emit_mla, need_ctx)
    return P.finish()


def build_s4s1(kind, need_ctx):
    P = Prog()
    xT = _scoped(P, "a_", emit_s4, kind, need_ctx)
    _scoped(P, "b_", emit_s1, xT)
    return P.finish()


def _pref(maps, pfx):
    return [{pfx + k: v for k, v in m.items()} for m in maps]


def _unpref(res, pfx):
    return [{k[len(pfx):]: v for k, v in r.items() if k.startswith(pfx)} for r in res]


def _merge(*lists):
    out = []
    for parts in zip(*lists):
        d = {}
        for p in parts:
            d.update(p)
        out.append(d)
    return out


def kernel(**inputs):
    inp = {k: np.asarray(v) for k, v in inputs.items()}
    x = np.ascontiguousarray(inp["x"], dtype=np.float32)
    xc = np.ascontiguousarray(inp["ctx"], dtype=np.float32)
    s1res = _run(_prog("s1", build_s1), s1_inputs(0, inp, x, xc))
    for l in range(2):
        need_ctx = l < 1
        hl, hc = _gather_fm(s1res, "o_hy")
        parts = [_pref(gla_inputs(s1res, inp, l), "g_"), _pref(hyena_inputs(hl, inp, l, SEQ), "h_")]
        if need_ctx:
            parts.append(_pref(hyena_inputs(hc, inp, l, CTX), "c_"))
        parts.append(_pref(mla_inputs(s1res, need_ctx), "m_"))
        mres = _run(_prog("mix", build_mix, need_ctx), _merge(*parts))
        glares = _unpref(mres, "g_"); hyres = _unpref(mres, "h_"); mlares = _unpref(mres, "m_")
        hycres = _unpref(mres, "c_") if need_ctx else None
        s4m = _pref(s4_inputs(l, inp, x, xc, s1res, glares, hyres, hycres, mlares, need_ctx), "a_")
        if l == 0:
            s1m = s1_inputs(1, inp, x, xc)
            for m in s1m:
                m.pop("x")
            res = _run(_prog("s4s1", build_s4s1, "dense", True), _merge(s4m, _pref(s1m, "b_")))
            s1res = _unpref(res, "b_")
            s4res = _unpref(res, "a_")
            xn = np.empty_like(x); xcn = np.empty((BATCH * CTX, D), np.float32)
            for c in range(NCORES):
                b, j = c // 4, c % 4
                o = s4res[c]["o_xT"]
                xn[b, RL * j:RL * (j + 1)] = o[:, :RL].T
                xcn[RC * c:RC * (c + 1)] = o[:, RL:].T
            x, xc = xn, xcn.reshape(BATCH, CTX, D)
        else:
            s4res = _unpref(_run(_prog("s4", lambda: _build_pref_s4()), s4m), "a_")
            out = np.empty_like(x)
            for c in range(NCORES):
                b, j = c // 4, c % 4
                out[b, RL * j:RL * (j + 1)] = s4res[c]["o_xT"].T
            return out


def _build_pref_s4():
    P = Prog()
    _scoped(P, "a_", emit_s4, "moe", False)
    return P.finish()
```

```python
import numpy as np
import ml_dtypes
from contextlib import ExitStack

import concourse.bass as bass
import concourse.mybir as mybir
from concourse.bass_utils import run_bass_kernel_spmd

F32 = mybir.dt.float32
BF16 = mybir.dt.bfloat16
AF = mybir.ActivationFunctionType
ALU = mybir.AluOpType
AX = mybir.AxisListType

NCORES = 8
D = 1024
SEQ = 16384
BATCH = 2
CTX = 256
ENGS = ("pe", "act", "dve", "pool", "sp")
SEM_EPOCH = 30000


class Prog:
    def __init__(self):
        self.nc = bass.Bass("TRN2", target_bir_lowering=False)
        self.es = ExitStack()
        self.sem_es = ExitStack()
        self.pfx = ""
        self.ops = {e: [] for e in ENGS}
        self.nsem = 0
        self.sem = {}
        self.cnt = {}
        for e in ENGS:
            self.sem[e] = self._newsem("c_" + e)
            self.cnt[e] = 0
        self.known = {e: {} for e in ENGS}
        self.bufs = {}
        self.dsem = {}
        self.dcount = {}
        self.n_ps = 0
        self.out_events = []

    def _newsem(self, name):
        self.nsem += 1
        s = self.sem_es.enter_context(self.nc.semaphore(f"{name}_{self.nsem}"))
        return (s, f"{name}_{self.nsem}")

    def din(self, name, shape, dt=F32):
        return self.nc.dram_tensor(self.pfx + name, list(shape), dt, kind="ExternalInput").ap()

    def dout(self, name, shape, dt=F32):
        return self.nc.dram_tensor(self.pfx + name, list(shape), dt, kind="ExternalOutput").ap()

    def dscr(self, name, shape, dt=F32):
        return self.nc.dram_tensor(self.pfx + name, list(shape), dt, kind="Internal").ap()

    def sb(self, name, shape, dt=F32):
        return self.es.enter_context(self.nc.sbuf_tensor("sb_" + self.pfx + name, list(shape), dt))

    def ps(self, name, shape, dt=F32):
        return self.es.enter_context(self.nc.psum_tensor("pp_" + self.pfx + name, list(shape), dt))

    def _collect(self, eng, r, w, is_dma=False):
        need = []
        for k in r:
            st = self.bufs.get(k)
            if st and st["w"] is not None:
                need.append((st["w"], "raw"))
            if st:
                for ev in st.get("wd", {}).values():
                    need.append((ev, "raw"))
        for k in w:
            st = self.bufs.get(k)
            if st:
                if st["w"] is not None:
                    need.append((st["w"], "waw"))
                for ev in st.get("wd", {}).values():
                    need.append((ev, "waw"))
                for ev in st["r"].values():
                    need.append((ev, "war"))
        waits = {}
        for (ev, kind) in need:
            (sem, semname), val, src, srcdma = ev
            if srcdma:
                if is_dma and kind == "waw":
                    continue
                val = self.dcount[semname]
            else:
                if src == eng and not is_dma:
                    if eng == "pe":
                        continue
                    if kind != "raw":
                        continue
            if self.known[eng].get(semname, 0) >= val:
                continue
            if semname not in waits or waits[semname][1] < val:
                waits[semname] = (sem, val)
        for semname, (sem, val) in waits.items():
            self.known[eng][semname] = val
        return list(waits.values())

    def _record(self, ev, eng, r, w):
        for k in w:
            wd = {}
            if ev[3]:
                old = self.bufs.get(k)
                if old:
                    wd = dict(old.get("wd", {}))
                    if old["w"] is not None and old["w"][3]:
                        wd[old["w"][3]] = old["w"]
                wd.pop(ev[3], None)
            self.bufs[k] = {"w": ev, "r": {}, "wd": wd}
        for k in r:
            st = self.bufs.setdefault(k, {"w": None, "r": {}, "wd": {}})
            tag = ev[3] if ev[3] else eng
            st["r"][tag] = ev

    def op(self, eng, fn, r=(), w=()):
        w = list(w) + [k for k in r if isinstance(k, str) and k.startswith("ps") and k not in w]
        waits = self._collect(eng, r, w)
        if self.cnt[eng] >= SEM_EPOCH:
            self.sem[eng] = self._newsem("c_" + eng)
            self.cnt[eng] = 0
        self.cnt[eng] += 1
        sem = self.sem[eng]
        ev = (sem, self.cnt[eng], eng, None)

        def run(e, waits=waits, sem=sem[0], fn=fn):
            for (s, v) in waits:
                e.wait_ge(s, v)
            fn(e).then_inc(sem, 1)

        self.ops[eng].append(run)
        self._record(ev, eng, r, w)
        return ev

    def dma(self, eng, out, in_, sk, r=(), w=(), is_out=False):
        waits = self._collect(eng, r, w, is_dma=True)
        if sk not in self.dsem or self.dsem[sk][1] >= SEM_EPOCH:
            self.dsem[sk] = [self._newsem("d"), 0]
        ds = self.dsem[sk]
        ds[1] += 16
        self.dcount[ds[0][1]] = ds[1]
        ev = (ds[0], ds[1], eng, sk)

        def run(e, waits=waits, sem=ds[0][0], out=out, in_=in_):
            for (s, v) in waits:
                e.wait_ge(s, v)
            e.dma_start(out=out, in_=in_).then_inc(sem, 16)

        self.ops[eng].append(run)
        self._record(ev, eng, r, w)
        if is_out:
            self.out_events.append(sk)
        return ev

    def barrier(self):
        targets = [(self.sem[e], self.cnt[e]) for e in ENGS if self.cnt[e] > 0]
        for (semt, val) in [(v[0], v[1]) for v in self.dsem.values()]:
            targets.append((semt, val))
        for e in ENGS:
            waits = []
            for (sem, semname), val in targets:
                if self.known[e].get(semname, 0) >= val:
                    continue
                self.known[e][semname] = val
                waits.append((sem, val))

            def run(eng, waits=waits):
                for (s, v) in waits:
                    eng.wait_ge(s, v)

            self.ops[e].append(run)
        self.bufs = {}

    def push_scope(self):
        self._outer_es = getattr(self, "_outer_es", [])
        self._outer_es.append(self.es)
        self.es = ExitStack()

    def pop_scope(self):
        self.barrier()
        self.es.close()
        self.es = self._outer_es.pop()

    def finish(self):
        waits = []
        for sk in dict.fromkeys(self.out_events):
            (sem, semname), val = self.dsem[sk]
            waits.append((sem, val))

        def run(e, waits=waits):
            for (s, v) in waits:
                e.wait_ge(s, v)

        self.ops["sp"].append(run)
        nc = self.nc
        ops = self.ops
        with nc.Block() as block:
            @block.sync
            def _(e):
                for f in ops["sp"]:
                    f(e)

            @block.tensor
            def _(e):
                for f in ops["pe"]:
                    f(e)

            @block.scalar
            def _(e):
                for f in ops["act"]:
                    f(e)

            @block.vector
            def _(e):
                for f in ops["dve"]:
                    f(e)

            @block.gpsimd
            def _(e):
                for f in ops["pool"]:
                    f(e)
        self.es.close()
        self.sem_es.close()
        return nc


def mm(P, out, lhsT, rhs, start, stop, r, w):
    return P.op("pe", lambda e: e.matmul(out, lhsT, rhs, start=start, stop=stop), r=r, w=w)


def act(P, out, in_, func, r, w, bias=0.0, scale=1.0, accum_out=None):
    if accum_out is None:
        return P.op("act", lambda e: e.activation(out, in_, func, bias=bias, scale=scale), r=r, w=w)
    return P.op("act", lambda e: e.activation(out, in_, func, bias=bias, scale=scale,
                                              accum_out=accum_out), r=r, w=w)


def tt(P, eng, out, in0, in1, op, r, w):
    return P.op(eng, lambda e: e.tensor_tensor(out, in0, in1, op), r=r, w=w)


def ts(P, eng, out, in0, s1, s2, op0, op1, r, w):
    if s2 is None:
        return P.op(eng, lambda e: e.tensor_scalar(out, in0, s1, None, op0), r=r, w=w)
    return P.op(eng, lambda e: e.tensor_scalar(out, in0, s1, s2, op0, op1), r=r, w=w)


def stt(P, out, in0, scalar, in1, op0, op1, r, w):
    return P.op("dve", lambda e: e.scalar_tensor_tensor(out, in0, scalar, in1, op0, op1), r=r, w=w)


def cp(P, eng, out, in_, r, w):
    if eng == "act":
        return P.op("act", lambda e: e.copy(out, in_), r=r, w=w)
    return P.op(eng, lambda e: e.tensor_copy(out, in_), r=r, w=w)


def recip(P, out, in_, r, w):
    return P.op("dve", lambda e: e.reciprocal(out, in_), r=r, w=w)


class Stage:
    def __init__(self, P, name, shape, dt, n):
        self.t = [P.sb(f"{name}{i}", shape, dt) for i in range(n)]
        self.k = [f"{name}{i}" for i in range(n)]
        self.i = 0

    def get(self):
        i = self.i
        self.i = (self.i + 1) % len(self.t)
        return self.t[i], self.k[i]


class Banks:
    def __init__(self, P, n=8):
        self.t = [P.ps(f"psb{i}", [128, 512]) for i in range(n)]
        self.i = 0
        self.n = n

    def get(self):
        i = self.i
        self.i = (self.i + 1) % self.n
        return self.t[i], f"psb{i}"


RL = SEQ * BATCH // NCORES
RC = CTX * BATCH // NCORES
RR = RL + RC
NX = 2176
C_Q, C_K, C_VG, C_ALR, C_HY, C_CQ, C_CKV, C_KRP, C_KRS = 0, 128, 256, 768, 800, 1568, 1824, 1984, 2080
PERM = np.concatenate([np.arange(8, 16), np.arange(0, 8), np.arange(24, 32), np.arange(16, 24)])


def row_groups():
    g = [(512 * i, 512, 0) for i in range(RL // 512)]
    g.append((RL, RC, 1))
    return g


def emit_mod(P, B, ccT, wmod, bmodT, modT, o_mod=None):
    cs = P.sb("mod_cs", [128, 8, 2])
    sT = P.sb("mod_sT", [128, 8, 2])
    bm = P.sb("mod_bm", [128, 48])
    P.dma("sp", cs[:], ccT, ("mod_cs", "ld"), w=["mod_cs"])
    P.dma("sp", bm[:], bmodT, ("mod_bm", "ld"), w=["mod_bm"])
    act(P, sT[:], cs[:], AF.Silu, r=["mod_cs"], w=["mod_sT"])
    wv = wmod.rearrange("(k p) n -> p k n", p=128)
    wt = [P.sb(f"mod_w{i}", [128, 8, 256]) for i in range(2)]
    for cg in range(24):
        s = cg % 2
        P.dma("sp", wt[s][:], wv[:, :, cg * 256:(cg + 1) * 256], (f"mod_w{s}", "ld"), w=[f"mod_w{s}"])
        for j in range(2):
            oc = cg * 2 + j
            bank, bk = B.get()
            for k in range(8):
                mm(P, bank[:, 0:2], wt[s][:, k, j * 128:(j + 1) * 128], sT[:, k, :],
                   start=(k == 0), stop=(k == 7), r=[f"mod_w{s}", "mod_sT"], w=[bk])
            act(P, modT[:, oc, :], bank[:, 0:2], AF.Identity, r=[bk, "mod_bm"], w=["modT"],
                bias=bm[:, oc:oc + 1])
    if o_mod is not None:
        P.dma("sp", o_mod, modT[:], ("modT", "st"), r=["modT"], is_out=True)


def emit_load_xT(P, B, xd, row0, nrow, ident, xt_slots, gi, evac):
    nt = (nrow + 127) // 128
    for t in range(nt):
        rows = min(128, nrow - 128 * t)
        s = (gi * 4 + t) % len(xt_slots)
        xt = xt_slots[s]
        key = f"xt{s}"
        P.dma("sp", xt[:rows, :], xd[row0 + 128 * t: row0 + 128 * t + rows, :], (key, "ld"), w=[key])
        for half in range(2):
            bank, bk = B.get()
            for kk in range(4):
                k = half * 4 + kk
                P.op("pe", lambda e, bank=bank, kk=kk, k=k, rows=rows, xt=xt: e.transpose(
                    bank[:, kk * 128: kk * 128 + rows], xt[:rows, k * 128:(k + 1) * 128],
                    ident[:rows, :rows]), r=[key, "ident"], w=[bk])
            for kk in range(4):
                k = half * 4 + kk
                evac(k, bank[:, kk * 128: kk * 128 + rows], bk, 128 * t, rows)


def build_s1():
    P = Prog()
    emit_s1(P)
    return P.finish()


def emit_s1(P, x_fm=None):
    B = Banks(P)
    xd = P.din("x", [RR, D]) if x_fm is None else None
    ccT = P.din("ccT", [128, 8, 2])
    wmod = P.din("wmod", [D, 6 * D])
    bmodT = P.din("bmodT", [128, 48])
    win = P.din("win", [D, NX])
    identd = P.din("ident", [128, 128])
    onesd = P.din("ones", [128, 128])
    wgd = P.din("wg", [32, 2, 128])
    bgd = P.din("bg", [128, 2])
    qgd = P.din("qg", [128, 2])
    kvgd = P.din("kvg", [128, 1])
    wuqd = P.din("wuq", [256, 768])
    wuqsd = P.din("wuqs", [256, 768])
    wukvd = P.din("wukv", [128, 1024])
    ropecd = P.din("ropec", [96, RR])
    ropesd = P.din("ropes", [96, RR])

    o_mod = P.dout("o_mod", [128, 48, 2])
    o_qk = P.dout("o_qk", [2, 128, RR])
    o_la = P.dout("o_la", [2, 128, RR])
    o_vg = P.dout("o_vg", [RR, 512])
    o_hy = P.dout("o_hy", [768, RR])
    o_QT = P.dout("o_QT", [8, 96, RR], BF16)
    o_KnT = P.dout("o_KnT", [8, 64, RR], BF16)
    o_kr = P.dout("o_kr", [32, RR], BF16)
    o_V = P.dout("o_V", [RR, 512], BF16)

    ident = P.sb("ident", [128, 128])
    ones = P.sb("ones", [128, 128])
    P.dma("sp", ident[:], identd, ("ident", "ld"), w=["ident"])
    P.dma("sp", ones[:], onesd, ("ones", "ld"), w=["ones"])
    modT = P.sb("modT", [128, 48, 2])
    emit_mod(P, B, ccT, wmod, bmodT, modT, o_mod)
    sc1p = P.sb("sc1p", [128, 8, 2])
    ts(P, "dve", sc1p[:], modT[:, 8:16, :], 1.0, None, ALU.add, None, r=["modT"], w=["sc1p"])

    wx = P.sb("wx", [128, 8, NX], BF16)
    winv = win.rearrange("(k p) n -> p k n", p=128)
    for k in range(8):
        P.dma("pool", wx[:, k, :], winv[:, k, :], ("wx", "ld"), w=["wx"])
    wg = P.sb("wg", [32, 2, 128]); bg = P.sb("bg", [128, 2]); qg = P.sb("qg", [128, 2]); kvg = P.sb("kvg", [128, 1])
    P.dma("sp", wg[:], wgd, ("wg", "ld"), w=["wg"])
    P.dma("sp", bg[:], bgd, ("bg", "ld"), w=["bg"])
    P.dma("sp", qg[:], qgd, ("qg", "ld"), w=["qg"])
    P.dma("sp", kvg[:], kvgd, ("kvg", "ld"), w=["kvg"])
    wuq = P.sb("wuq", [128, 2, 768], BF16); wuqs = P.sb("wuqs", [128, 2, 768], BF16)
    wukv = P.sb("wukv", [128, 1024], BF16)
    P.dma("pool", wuq[:], wuqd.rearrange("(k p) n -> p k n", p=128), ("wuq", "ld"), w=["wuq"])
    P.dma("pool", wuqs[:], wuqsd.rearrange("(k p) n -> p k n", p=128), ("wuqs", "ld"), w=["wuqs"])
    P.dma("pool", wukv[:], wukvd, ("wukv", "ld"), w=["wukv"])
    ropecS = Stage(P, "ropec", [96, 512], F32, 2)
    ropesS = Stage(P, "ropes", [96, 512], F32, 2)

    if x_fm is None:
        xt_slots = [P.sb(f"xt{i}", [128, D]) for i in range(3)]
    else:
        S_xf = Stage(P, "xfm", [128, 8, 512], F32, 1)
    NB = 2
    hT = [P.sb(f"hT{i}", [128, 8, 512], BF16) for i in range(NB)]
    st_qk = [P.sb(f"sqk{i}", [128, 2, 512]) for i in range(NB)]
    st_la = [P.sb(f"sla{i}", [128, 2, 512]) for i in range(NB)]
    st_hy = Stage(P, "shy", [128, 512], F32, 3)
    st_vg = Stage(P, "svg", [128, 512], F32, 3)
    st_alr = [P.sb(f"salr{i}", [32, 512]) for i in range(NB)]
    st_cq = [P.sb(f"scq{i}", [128, 3, 512]) for i in range(1)] * 2
    st_sq = [P.sb(f"ssq{i}", [128, 3, 512]) for i in range(1)] * 2
    st_rs = [P.sb(f"srs{i}", [128, 2, 512]) for i in range(1)] * 2
    st_cn = [P.sb(f"scn{i}", [128, 3, 512], BF16) for i in range(NB)]
    st_QT = Stage(P, "sQT", [96, 512], BF16, 4)
    st_Kn = Stage(P, "sKn", [64, 512], BF16, 4)
    st_kr = [P.sb(f"skr{i}", [96, 512], BF16) for i in range(NB)]
    st_V = Stage(P, "sV", [128, 512], BF16, 3)
    tmpA = [P.sb(f"tmpA{i}", [96, 512]) for i in range(2)]
    tmpB = [P.sb(f"tmpB{i}", [96, 512]) for i in range(2)]
    gz = [P.sb(f"gz{i}", [128, 512]) for i in range(2)]
    ga = [P.sb(f"ga{i}", [128, 512]) for i in range(2)]
    gm = [P.sb(f"gm{i}", [128, 512]) for i in range(2)]
    tcount = [0]

    for gi, (row0, ntok, msel) in enumerate(row_groups()):
        s = gi % NB
        N = ntok
        hk = f"hT{s}"

        def evac(k, pap, bk, col0, rows, s=s, msel=msel, hk=hk):
            act(P, hT[s][:, k, col0:col0 + rows], pap, AF.Identity, r=[bk, "sc1p", "modT"], w=[hk],
                bias=modT[:, k, msel:msel + 1], scale=sc1p[:, k, msel:msel + 1])

        if x_fm is None:
            emit_load_xT(P, B, xd, row0, ntok, ident, xt_slots, gi, evac)
        else:
            xf, xfk = S_xf.get()
            P.dma("sp", xf[:, :, :N], x_fm.rearrange("(k p) n -> p k n", p=128)[:, :, row0:row0 + N], (xfk, "ld"), w=[xfk])
            for k in range(8):
                evac(k, xf[:, k, :N], xfk, 0, N)

        def fm(c0, M):
            bank, bk = B.get()
            for k in range(8):
                mm(P, bank[:M, :N], wx[:, k, c0:c0 + M], hT[s][:, k, :N], start=(k == 0), stop=(k == 7),
                   r=["wx", hk], w=[bk])
            return bank, bk

        for j, c0 in enumerate((C_Q, C_K)):
            bank, bk = fm(c0, 128)
            cp(P, "act", st_qk[s][:, j, :N], bank[:, :N], r=[bk], w=[f"sqk{s}"])
        for j in range(2):
            P.dma("sp", o_qk[j, :, row0:row0 + N], st_qk[s][:, j, :N], (f"sqk{s}", "st"), r=[f"sqk{s}"], is_out=True)
        for j in range(6):
            bank, bk = fm(C_HY + 128 * j, 128)
            stt_, sk_ = st_hy.get()
            cp(P, "dve" if j % 2 else "act", stt_[:, :N], bank[:, :N], r=[bk], w=[sk_])
            P.dma("sp", o_hy[128 * j:128 * (j + 1), row0:row0 + N], stt_[:, :N], (sk_, "st"), r=[sk_], is_out=True)
        bank, bk = fm(C_ALR, 32)
        cp(P, "dve", st_alr[s][:, :N], bank[:32, :N], r=[bk], w=[f"salr{s}"])
        for d in range(2):
            u = tcount[0] % 2
            tcount[0] += 1
            bank, bk = B.get()
            mm(P, bank[:, :N], wg[:, d, :], st_alr[s][:, :N], start=True, stop=True, r=["wg", f"salr{s}"], w=[bk])
            act(P, gz[u][:, :N], bank[:, :N], AF.Identity, r=[bk, "bg"], w=[f"gz{u}"], bias=bg[:, d:d + 1])
            act(P, ga[u][:, :N], gz[u][:, :N], AF.Abs, r=[f"gz{u}"], w=[f"ga{u}"])
            act(P, ga[u][:, :N], ga[u][:, :N], AF.Exp, r=[f"ga{u}"], w=[f"ga{u}"], scale=-1.0)
            act(P, ga[u][:, :N], ga[u][:, :N], AF.Ln, r=[f"ga{u}"], w=[f"ga{u}"], bias=1.0)
            ts(P, "dve", gm[u][:, :N], gz[u][:, :N], 0.0, 1.0 / 16.0, ALU.min, ALU.mult, r=[f"gz{u}"], w=[f"gm{u}"])
            stt(P, st_la[s][:, d, :N], ga[u][:, :N], -1.0 / 16.0, gm[u][:, :N], ALU.mult, ALU.add,
                r=[f"ga{u}", f"gm{u}"], w=[f"sla{s}"])
        for d in range(2):
            P.dma("sp", o_la[d, :, row0:row0 + N], st_la[s][:, d, :N], (f"sla{s}", "st"), r=[f"sla{s}"], is_out=True)
        nt = (N + 127) // 128
        for t in range(nt):
            rows = min(128, N - 128 * t)
            bank, bk = B.get()
            for k in range(8):
                mm(P, bank[:rows, :], hT[s][:, k, 128 * t:128 * t + rows], wx[:, k, C_VG:C_VG + 512],
                   start=(k == 0), stop=(k == 7), r=["wx", hk], w=[bk])
            stt_, sk_ = st_vg.get()
            cp(P, "dve", stt_[:rows, :], bank[:rows, :], r=[bk], w=[sk_])
            P.dma("sp", o_vg[row0 + 128 * t: row0 + 128 * t + rows, :], stt_[:rows, :], (sk_, "st"),
                  r=[sk_], is_out=True)
        for j, c0 in enumerate((C_CQ, C_CQ + 128, C_CKV)):
            bank, bk = fm(c0, 128)
            cp(P, "dve", st_cq[s][:, j, :N], bank[:, :N], r=[bk], w=["scq0"])
            act(P, st_sq[s][:, j, :N], bank[:, :N], AF.Square, r=[bk], w=["ssq0"])
        for j, (chunks, nfeat) in enumerate((((0, 1), 256.0), ((2,), 128.0))):
            bank, bk = B.get()
            for i, c in enumerate(chunks):
                mm(P, bank[:, :N], ones[:], st_sq[s][:, c, :N], start=(i == 0), stop=(i == len(chunks) - 1),
                   r=["ones", "ssq0"], w=[bk])
            act(P, st_rs[s][:, j, :N], bank[:, :N], AF.Sqrt, r=[bk], w=["srs0"], bias=1e-6, scale=1.0 / nfeat)
            recip(P, st_rs[s][:, j, :N], st_rs[s][:, j, :N], r=["srs0"], w=["srs0"])
        for c in range(3):
            j = 0 if c < 2 else 1
            gsc = qg[:, c:c + 1] if c < 2 else kvg[:, 0:1]
            stt(P, st_cn[s][:, c, :N], st_cq[s][:, c, :N], gsc, st_rs[s][:, j, :N], ALU.mult, ALU.mult,
                r=["scq0", "srs0", "qg", "kvg"], w=[f"scn{s}"])
        cs = slice(row0, row0 + N)
        ropec, rck = ropecS.get()
        ropes, rsk = ropesS.get()
        P.dma("sp", ropec[:, :N], ropecd[:, cs], (rck, "ld"), w=[rck])
        P.dma("sp", ropes[:, :N], ropesd[:, cs], (rsk, "ld"), w=[rsk])
        for h in range(8):
            b1, k1 = B.get()
            b2, k2 = B.get()
            for kk in range(2):
                mm(P, b1[:96, :N], wuq[:, kk, h * 96:(h + 1) * 96], st_cn[s][:, kk, :N], start=(kk == 0), stop=(kk == 1),
                   r=["wuq", f"scn{s}"], w=[k1])
            for kk in range(2):
                mm(P, b2[:96, :N], wuqs[:, kk, h * 96:(h + 1) * 96], st_cn[s][:, kk, :N], start=(kk == 0), stop=(kk == 1),
                   r=["wuqs", f"scn{s}"], w=[k2])
            u = tcount[0] % 2
            tcount[0] += 1
            sq_, sqk_ = st_QT.get()
            cp(P, "act", sq_[0:64, :N], b1[0:64, :N], r=[k1], w=[sqk_])
            tt(P, "dve", tmpA[u][64:96, :N], b1[64:96, :N], ropec[64:96, :N], ALU.mult, r=[k1, rck], w=[f"tmpA{u}"])
            tt(P, "dve", tmpB[u][64:96, :N], b2[64:96, :N], ropes[64:96, :N], ALU.mult, r=[k2, rsk], w=[f"tmpB{u}"])
            tt(P, "pool", sq_[64:96, :N], tmpA[u][64:96, :N], tmpB[u][64:96, :N], ALU.add,
               r=[f"tmpA{u}", f"tmpB{u}"], w=[sqk_])
            P.dma("sp", o_QT[h, :, cs], sq_[:, :N], (sqk_, "st"), r=[sqk_], is_out=True)
        for h in range(8):
            bank, bk = B.get()
            mm(P, bank[:64, :N], wukv[:, h * 128:h * 128 + 64], st_cn[s][:, 2, :N], start=True, stop=True,
               r=["wukv", f"scn{s}"], w=[bk])
            sk2_, skk_ = st_Kn.get()
            cp(P, "act", sk2_[:, :N], bank[:64, :N], r=[bk], w=[skk_])
            P.dma("sp", o_KnT[h, :, cs], sk2_[:, :N], (skk_, "st"), r=[skk_], is_out=True)
        b1, k1 = fm(C_KRP, 96)
        b2, k2 = fm(C_KRS, 96)
        u = tcount[0] % 2
        tcount[0] += 1
        tt(P, "dve", tmpA[u][64:96, :N], b1[64:96, :N], ropec[64:96, :N], ALU.mult, r=[k1, rck], w=[f"tmpA{u}"])
        tt(P, "dve", tmpB[u][64:96, :N], b2[64:96, :N], ropes[64:96, :N], ALU.mult, r=[k2, rsk], w=[f"tmpB{u}"])
        tt(P, "pool", st_kr[s][64:96, :N], tmpA[u][64:96, :N], tmpB[u][64:96, :N], ALU.add,
           r=[f"tmpA{u}", f"tmpB{u}"], w=[f"skr{s}"])
        P.dma("sp", o_kr[:, cs], st_kr[s][64:96, :N], (f"skr{s}", "st"), r=[f"skr{s}"], is_out=True)
        wv = wukv[:].rearrange("p (h c) -> p h c", c=128)[:, :, 64:128]
        for t in range(nt):
            rows = min(128, N - 128 * t)
            bank, bk = B.get()
            mm(P, bank[:rows, :].rearrange("p (h c) -> p h c", c=64), st_cn[s][:, 2, 128 * t:128 * t + rows], wv,
               start=True, stop=True, r=["wukv", f"scn{s}"], w=[bk])
            sv_, svk_ = st_V.get()
            cp(P, "act", sv_[:rows, :], bank[:rows, :], r=[bk], w=[svk_])
            P.dma("sp", o_V[row0 + 128 * t: row0 + 128 * t + rows, :], sv_[:rows, :], (svk_, "st"),
                  r=[svk_], is_out=True)
    return None


def _fm(v):
    v = np.asarray(v, np.float32)
    return np.ascontiguousarray(v.reshape(-1, 128).T)


def _rope_tables():
    t = np.arange(SEQ)
    row = (t // 64).astype(np.float32)
    col = (t % 64).astype(np.float32)
    inv = (np.float32(10000.0) ** (-np.arange(0, 16, 2, dtype=np.float32) / np.float32(16))).astype(np.float32)
    c = np.zeros((32, SEQ), np.float32)
    s = np.zeros((32, SEQ), np.float32)
    for i in range(32):
        pos = row if i < 16 else col
        ang = (pos * inv[i % 8]).astype(np.float32)
        c[i] = np.cos(ang)
        sg = -1.0 if (i % 16) < 8 else 1.0
        s[i] = sg * np.sin(ang)
    return c, s


_CONST = {}


def _consts():
    if not _CONST:
        _CONST["ident"] = np.eye(128, dtype=np.float32)
        _CONST["ones"] = np.ones((128, 128), np.float32)
        _CONST["rope"] = _rope_tables()
    return _CONST


def s1_inputs(l, inp, x_cur, xc_cur):
    C = _consts()
    rc, rs = C["rope"]
    w_in = inp["w_in"][l]
    win = np.zeros((D, NX), np.float32)
    win[:, :1984] = w_in
    win[:, C_KRP + 64:C_KRP + 96] = w_in[:, 1952:1984]
    win[:, C_KRS + 64:C_KRS + 96] = w_in[:, 1952:1984][:, PERM]
    wg = np.zeros((32, 2, 128), np.float32)
    for d in range(2):
        wg[16 * d:16 * d + 16, d, :] = inp["gla_w_gate"][l][d]
    bg = np.ascontiguousarray(inp["gla_b_gate"][l].T)
    wuq = inp["mla_w_uq"][l]
    wuqs = np.zeros_like(wuq)
    for h in range(8):
        wuqs[:, h * 96 + 64:h * 96 + 96] = wuq[:, h * 96 + 64:h * 96 + 96][:, PERM]
    xcf = xc_cur.reshape(BATCH * CTX, D)
    maps = []
    for c in range(NCORES):
        b, j = c // 4, c % 4
        xr = np.concatenate([x_cur[b, RL * j:RL * (j + 1)], xcf[RC * c:RC * (c + 1)]], 0)
        cc = np.stack([inp["c"][b], inp["c_ctx"]], 0)
        ccT = np.ascontiguousarray(cc.reshape(2, 8, 128).transpose(2, 1, 0))
        ropec = np.zeros((96, RR), np.float32)
        ropes = np.zeros((96, RR), np.float32)
        ropec[64:, :RL] = rc[:, RL * j:RL * (j + 1)]
        ropes[64:, :RL] = rs[:, RL * j:RL * (j + 1)]
        ropec[64:, RL:] = 1.0
        maps.append({
            "x": np.ascontiguousarray(xr, dtype=np.float32), "ccT": ccT,
            "wmod": inp["w_mod"][l], "bmodT": _fm(inp["b_mod"][l]), "win": win,
            "ident": C["ident"], "ones": C["ones"], "wg": wg, "bg": bg,
            "qg": _fm(inp["mla_q_norm_g"][l]), "kvg": _fm(inp["mla_kv_norm_g"][l]),
            "wuq": wuq, "wuqs": wuqs, "wukv": inp["mla_w_ukv"][l],
            "ropec": ropec, "ropes": ropes,
        })
    return maps


GC = 64
TA = CTX + SEQ + CTX
NCH = TA // GC
NSB = NCH // 8


def scan_add(P, out, data0, data1, r, w):
    return P.op("dve", lambda e: e.tensor_tensor_scan(out, data0, data1, 0.0, ALU.mult, ALU.add), r=r, w=w)


def build_gla():
    P = Prog()
    emit_gla(P)
    return P.finish()


def emit_gla(P):
    B = Banks(P)
    qqd = P.din("qq", [64, TA]); kkd = P.din("kk", [64, TA]); lad = P.din("la2", [64, TA])
    vvd = P.din("vv", [64, NCH, 64]); ggd = P.din("gg", [64, NCH, 64])
    gGd = P.din("gG", [64, 8, 64]); mFd = P.din("mF", [64, 8, 64]); mBd = P.din("mB", [64, 8, 64])
    smd = P.din("smask", [64, 512]); identd = P.din("ident", [128, 128])
    o_ya = P.dout("o_ya", [64, TA])

    ident = P.sb("ident", [128, 128]); gG = P.sb("gG", [64, 8, 64]); mF = P.sb("mF", [64, 8, 64]); mB = P.sb("mB", [64, 8, 64])
    smask = P.sb("smask", [64, 512])
    for t, d_, k in ((ident, identd, "ident"), (gG, gGd, "gG"), (mF, mFd, "mF"), (mB, mBd, "mB"), (smask, smd, "smask")):
        P.dma("sp", t[:], d_, (k, "ld"), w=[k])
    KV = P.sb("gKV", [64, NCH, 64]); X = P.sb("gX", [64, NCH, 64]); Dall = P.sb("gD", [64, NCH])
    octx = P.sb("octx", [64, 4, 64])
    S_la = Stage(P, "g_la", [64, 512], F32, 2); S_q = Stage(P, "g_q", [64, 512], F32, 2); S_k = Stage(P, "g_k", [64, 512], F32, 2)
    S_p = Stage(P, "g_p", [64, 512], F32, 2); S_u = Stage(P, "g_u", [64, 512], F32, 2)
    S_eq = Stage(P, "g_eq", [64, 512], F32, 2); S_ek = Stage(P, "g_ek", [64, 512], F32, 2)
    S_kt = Stage(P, "g_kt", [64, 8, 64], F32, 2); S_v = Stage(P, "g_v", [64, 8, 64], F32, 2); S_g = Stage(P, "g_g", [64, 8, 64], F32, 2)
    S_at = Stage(P, "g_at", [64, 8, 64], F32, 2); S_a2 = Stage(P, "g_a2", [64, 8, 64], F32, 2)
    S_o = Stage(P, "g_o", [64, 8, 64], F32, 2); S_o2 = Stage(P, "g_o2", [64, 8, 64], F32, 2)
    S_st = Stage(P, "g_st", [64, 8], F32, 2); S_y = Stage(P, "g_y", [64, 512], F32, 2)

    def prep(sb, need_q):
        cs = slice(512 * sb, 512 * sb + 512)
        la, lak = S_la.get(); kk, kkk = S_k.get()
        P.dma("sp", la[:], lad[:, cs], (lak, "ld"), w=[lak])
        P.dma("sp", kk[:], kkd[:, cs], (kkk, "ld"), w=[kkk])
        p, pk = S_p.get(); u, uk = S_u.get()
        scan_add(P, p[:], smask[:], la[:], r=["smask", lak], w=[pk])
        cp(P, "pool", u[0:32, :], p[0:32, :], r=[pk], w=[uk])
        tt(P, "pool", u[32:64, :], la[32:64, :], p[32:64, :], ALU.subtract, r=[lak, pk], w=[uk])
        act(P, Dall[:, 8 * sb:8 * sb + 8], p[:].rearrange("p (c t) -> p c t", t=64)[:, :, 63], AF.Exp, r=[pk], w=["gD"])
        ek, ekk = S_ek.get()
        act(P, ek[:], u[:], AF.Exp, r=[uk], w=[ekk], scale=-1.0)
        tt(P, "dve", ek[:], ek[:], kk[:], ALU.mult, r=[ekk, kkk], w=[ekk])
        qd = None; qdk = None
        if need_q:
            qq, qqk = S_q.get()
            P.dma("sp", qq[:], qqd[:, cs], (qqk, "ld"), w=[qqk])
            eq, eqk = S_eq.get()
            act(P, eq[:], u[:], AF.Exp, r=[uk], w=[eqk])
            stt(P, eq[:], eq[:], float(32 ** -0.5), qq[:], ALU.mult, ALU.mult, r=[eqk, qqk], w=[eqk])
            qd, qdk = eq, eqk
        return ek, ekk, qd, qdk

    for sb in range(NSB):
        kd, kdk, _, _ = prep(sb, False)
        v, vk = S_v.get()
        P.dma("sp", v[:], vvd[:, 8 * sb:8 * sb + 8, :], (vk, "ld"), w=[vk])
        bank, bk = B.get()
        for c in range(8):
            P.op("pe", lambda e, o=bank[:64, 64 * c:64 * c + 64], i=kd[:, 64 * c:64 * c + 64], idn=ident[:64, :64]:
                 e.transpose(o, i, idn), r=[kdk, "ident"], w=[bk])
        kt, ktk = S_kt.get()
        cp(P, "act", kt[:].rearrange("p c f -> p (c f)"), bank[:64, :], r=[bk], w=[ktk])
        bank2, bk2 = B.get()
        for c in range(8):
            mm(P, bank2[:64, 64 * c:64 * c + 64], kt[:, c, :], v[:, c, :], True, True, r=[ktk, vk], w=[bk2])
        for c in range(8):
            ch = 8 * sb + c
            act(P, KV[0:32, ch, :], bank2[0:32, 64 * c:64 * c + 64], AF.Copy, r=[bk2, "gD"], w=["gKV"],
                scale=Dall[0:32, ch:ch + 1])
            if ch >= 1:
                act(P, KV[32:64, ch, :], bank2[32:64, 64 * c:64 * c + 64], AF.Copy, r=[bk2, "gD"], w=["gKV"],
                    scale=Dall[32:64, ch - 1:ch])
    P.op("dve", lambda e: e.memset(X[:, 0, :], 0.0), w=["gX"])
    P.op("dve", lambda e: e.memset(X[:, NCH - 1, :], 0.0), w=["gX"])
    for c in range(NCH - 1):
        stt(P, X[0:32, c + 1, :], X[0:32, c, :], Dall[0:32, c:c + 1], KV[0:32, c, :], ALU.mult, ALU.add,
            r=["gX", "gD", "gKV"], w=["gX"])
    for c in range(NCH - 2, -1, -1):
        stt(P, X[32:64, c, :], X[32:64, c + 1, :], Dall[32:64, c:c + 1], KV[32:64, c + 1, :], ALU.mult, ALU.add,
            r=["gX", "gD", "gKV"], w=["gX"])
    for sb in range(NSB):
        kd, kdk, qd, qdk = prep(sb, True)
        v, vk = S_v.get(); g, gk = S_g.get()
        P.dma("sp", v[:], vvd[:, 8 * sb:8 * sb + 8, :], (vk, "ld"), w=[vk])
        P.dma("sp", g[:], ggd[:, 8 * sb:8 * sb + 8, :], (gk, "ld"), w=[gk])
        bf_, bfk = B.get(); bb_, bbk = B.get()
        for c in range(8):
            cc = slice(64 * c, 64 * c + 64)
            mm(P, bf_[:64, cc], kd[0:32, cc], qd[0:32, cc], True, True, r=[kdk, qdk], w=[bfk])
            mm(P, bb_[:64, cc], kd[32:64, cc], qd[32:64, cc], True, True, r=[kdk, qdk], w=[bbk])
        at, atk = S_at.get(); a2, a2k = S_a2.get()
        tt(P, "dve", at[:].rearrange("p c f -> p (c f)"), bf_[:64, :], mF[:].rearrange("p c f -> p (c f)"), ALU.mult,
           r=[bfk, "mF"], w=[atk])
        tt(P, "dve", a2[:].rearrange("p c f -> p (c f)"), bb_[:64, :], mB[:].rearrange("p c f -> p (c f)"), ALU.mult,
           r=[bbk, "mB"], w=[a2k])
        tt(P, "pool", at[:], at[:], a2[:], ALU.add, r=[atk, a2k], w=[atk])
        ob, obk = B.get()
        for c in range(8):
            ch = 8 * sb + c
            cc = slice(64 * c, 64 * c + 64)
            mm(P, ob[:64, cc], at[:, c, :], v[:, c, :], True, False, r=[atk, vk], w=[obk])
            mm(P, ob[:64, cc], qd[:, cc], X[:, ch, :], False, True, r=[qdk, "gX"], w=[obk])
        o, ok = S_o.get(); o2, o2k = S_o2.get(); st, stk = S_st.get()
        cp(P, "act", o[:].rearrange("p c f -> p (c f)"), ob[:64, :], r=[obk], w=[ok])
        if sb == 0:
            cp(P, "dve", octx[:], o[:, 0:4, :], r=[ok], w=["octx"])
        if sb == NSB - 1:
            tt(P, "dve", o[:, 4:8, :], o[:, 4:8, :], octx[:], ALU.add, r=[ok, "octx"], w=[ok])
        tt(P, "dve", o2[:], o[:], o[:], ALU.mult, r=[ok], w=[o2k])
        P.op("dve", lambda e, o_=st[:], i_=o2[:]: e.tensor_reduce(o_, i_, AX.X, ALU.add), r=[o2k], w=[stk])
        act(P, st[:], st[:], AF.Sqrt, r=[stk], w=[stk], bias=1e-6, scale=1.0 / 64.0)
        recip(P, st[:], st[:], r=[stk], w=[stk])
        act(P, o2[:], g[:], AF.Silu, r=[gk], w=[o2k])
        tt(P, "pool", o2[:], o2[:], gG[:], ALU.mult, r=[o2k, "gG"], w=[o2k])
        for c in range(8):
            stt(P, o[:, c, :], o[:, c, :], st[:, c:c + 1], o2[:, c, :], ALU.mult, ALU.mult, r=[ok, stk, o2k], w=[ok])
        tb, tbk = B.get()
        for c in range(8):
            P.op("pe", lambda e, o_=tb[:64, 64 * c:64 * c + 64], i_=o[:, c, :], idn=ident[:64, :64]:
                 e.transpose(o_, i_, idn), r=[ok, "ident"], w=[tbk])
        y, yk = S_y.get()
        cp(P, "act", y[:], tb[:64, :], r=[tbk], w=[yk])
        P.dma("sp", o_ya[:, 512 * sb:512 * sb + 512], y[:], (yk, "st"), r=[yk], is_out=True)
    return None


def _gather_fm(s1res, name, sub=None):
    lat, ctx = [], []
    for b in range(BATCH):
        parts = [s1res[4 * b + j][name] if sub is None else s1res[4 * b + j][name][sub] for j in range(4)]
        lat.append(np.concatenate([p[..., :RL] for p in parts], -1))
        ctx.append(np.concatenate([p[..., RL:] for p in parts], -1))
    return lat, ctx


def _gather_tm(s1res, name):
    lat, ctx = [], []
    for b in range(BATCH):
        parts = [s1res[4 * b + j][name] for j in range(4)]
        lat.append(np.concatenate([p[:RL] for p in parts], 0))
        ctx.append(np.concatenate([p[RL:] for p in parts], 0))
    return lat, ctx


def gla_inputs(s1res, inp, l):
    C = _consts()
    if "mF" not in C:
        s = np.arange(64)[:, None]; t = np.arange(64)[None, :]
        C["mF"] = np.ascontiguousarray(np.broadcast_to((s <= t).astype(np.float32)[:, None, :], (64, 8, 64)))
        C["mB"] = np.ascontiguousarray(np.broadcast_to((s >= t).astype(np.float32)[:, None, :], (64, 8, 64)))
        sm = np.ones((64, 512), np.float32); sm[:, ::64] = 0.0
        C["smask"] = sm
    ql, qc = _gather_fm(s1res, "o_qk", 0)
    kl, kc = _gather_fm(s1res, "o_qk", 1)
    lfl, lfc = _gather_fm(s1res, "o_la", 0)
    lbl, lbc = _gather_fm(s1res, "o_la", 1)
    vgl, vgc = _gather_tm(s1res, "o_vg")
    gG = np.ascontiguousarray(np.broadcast_to(inp["gla_norm_g"][l][None, None, :], (64, 8, 64)), dtype=np.float32)
    maps = []
    for c in range(NCORES):
        b, h = c // 4, c % 4
        hs = slice(32 * h, 32 * h + 32)
        arr = lambda lat, ctx: np.concatenate([ctx, lat, ctx], -1)
        q = arr(ql[b][hs], qc[b][hs]); k = arr(kl[b][hs], kc[b][hs])
        qf = q.copy(); qf[:, CTX + SEQ:] = 0.0
        qb = q.copy(); qb[:, :CTX] = 0.0
        la2 = np.concatenate([arr(lfl[b][hs], lfc[b][hs]), arr(lbl[b][hs], lbc[b][hs])], 0)
        vg = np.concatenate([vgc[b], vgl[b], vgc[b]], 0)
        tm = lambda x: np.ascontiguousarray(x.reshape(NCH, 64, 64).transpose(1, 0, 2))
        maps.append({
            "qq": np.ascontiguousarray(np.concatenate([qf, qb], 0)), "kk": np.ascontiguousarray(np.concatenate([k, k], 0)),
            "la2": np.ascontiguousarray(la2), "vv": tm(vg[:, 64 * h:64 * h + 64]), "gg": tm(vg[:, 256 + 64 * h:256 + 64 * h + 64]),
            "gG": gG, "mF": C["mF"], "mB": C["mB"], "smask": C["smask"], "ident": C["ident"],
        })
    return maps


MAGIC = 12582912.0
TWO_PI = float(2.0 * np.pi)
PI_LO = 3.1415925


def sinr(P, out, x, xk, t1, t1k, outk):
    ts(P, "dve", t1, x, 1.0 / TWO_PI, MAGIC, ALU.mult, ALU.add, r=[xk], w=[t1k])
    ts(P, "dve", t1, t1, -MAGIC, None, ALU.add, None, r=[t1k], w=[t1k])
    stt(P, t1, t1, -TWO_PI, x, ALU.mult, ALU.add, r=[t1k, xk], w=[t1k])
    ts(P, "dve", t1, t1, -PI_LO, PI_LO, ALU.max, ALU.min, r=[t1k], w=[t1k])
    act(P, out, t1, AF.Sin, r=[t1k], w=[outk])


def build_hyena(L):
    P = Prog()
    emit_hyena(P, L)
    return P.finish()


def emit_hyena(P, L):
    NB = L // 128
    CB = min(512, L)
    NCB = L // CB
    B = Banks(P, 6)
    ybanks = [P.ps("psy0", [128, 512]), P.ps("psy1", [128, 512])]
    hyd = P.din("hy", [96, 2, L]); cwd = P.din("cw", [96, 4])
    ZTd = P.din("ZT", [33, L]); ZTrd = P.din("ZTr", [33, L])
    fw1d = P.din("fw1", [33, 64]); fv1d = P.din("fv1", [64, 2]); fw2d = P.din("fw2", [64, 64]); fv2d = P.din("fv2", [64, 2])
    fw3d = P.din("fw3c", [64, 2, 64]); fb3d = P.din("fb3c", [64, 2]); dnegd = P.din("dneg", [64, 1]); e0d = P.din("e0", [33, 64])
    skd = P.din("skip", [128, 2, 32]); identd = P.din("ident", [128, 128]); antid = P.din("anti", [128, 128])
    onesd = P.din("ones", [128, 128])
    o_yb = P.dout("o_yb", [32, 2, L])
    gs = P.dscr("gs_scratch", [64, 2 * L], BF16)
    ucd = P.dscr("uc_scratch", [96, 2, L], F32)

    def ld(name, shape, src, dt=F32):
        t = P.sb(name, shape, dt)
        P.dma("sp", t[:], src, (name, "ld"), w=[name])
        return t

    cw = ld("cw", [96, 4], cwd); fw1 = ld("fw1", [33, 64], fw1d); fv1 = ld("fv1", [64, 2], fv1d)
    fw2 = ld("fw2", [64, 64], fw2d); fv2 = ld("fv2", [64, 2], fv2d); fw3 = ld("fw3", [64, 2, 64], fw3d)
    fb3 = ld("fb3", [64, 2], fb3d); dneg = ld("dneg", [64, 1], dnegd); e0 = ld("e0", [33, 64], e0d)
    skip = ld("skip", [128, 2, 32], skd); ident = ld("ident", [128, 128], identd); anti = ld("anti", [128, 128], antid)
    ones = ld("ones", [128, 128], onesd)
    fbs = P.sb("fbs", [64, 2])
    tt(P, "dve", fbs[:, 0:1], fv1[:, 0:1], fv1[:, 1:2], ALU.mult, r=["fv1"], w=["fbs"])
    tt(P, "dve", fbs[:, 1:2], fv2[:, 0:1], fv2[:, 1:2], ALU.mult, r=["fv2"], w=["fbs"])

    ssq = P.sb("ssq", [64, 2 * NCB])
    S_z = Stage(P, "f_z", [33, CB], F32, 2)
    S_a = Stage(P, "f_a", [64, CB], F32, 2); S_t = Stage(P, "f_t", [64, CB], F32, 2)
    S_h1 = Stage(P, "f_h1", [64, CB], F32, 2); S_h2 = Stage(P, "f_h2", [64, CB], F32, 2)
    S_w = Stage(P, "f_w", [64, CB], F32, 2); S_hr = Stage(P, "f_hr", [64, CB], F32, 2)
    S_hb = Stage(P, "f_hb", [64, CB], BF16, 2); S_sq = Stage(P, "f_sq", [64, CB], F32, 2)
    for d in range(2):
        Zsrc = ZTrd if d == 0 else ZTd
        for blk in range(NCB):
            cs = slice(CB * blk, CB * blk + CB)
            z, zk = S_z.get()
            P.dma("sp", z[:], Zsrc[:, cs], (zk, "ld"), w=[zk])
            b1, k1 = B.get()
            mm(P, b1[:64, :CB], fw1[:], z[:], True, True, r=["fw1", zk], w=[k1])
            a, ak = S_a.get(); t1, t1k = S_t.get(); h1, h1k = S_h1.get()
            act(P, a[:], b1[:64, :CB], AF.Identity, r=[k1, "fv1", "fbs"], w=[ak], bias=fbs[:, 0:1], scale=fv1[:, 0:1])
            sinr(P, h1[:], a[:], ak, t1[:], t1k, h1k)
            b2, k2 = B.get()
            mm(P, b2[:64, :CB], fw2[:], h1[:], True, True, r=["fw2", h1k], w=[k2])
            a, ak = S_a.get(); t1, t1k = S_t.get(); h2, h2k = S_h2.get()
            act(P, a[:], b2[:64, :CB], AF.Identity, r=[k2, "fv2", "fbs"], w=[ak], bias=fbs[:, 1:2], scale=fv2[:, 0:1])
            sinr(P, h2[:], a[:], ak, t1[:], t1k, h2k)
            b3, k3 = B.get(); b4, k4 = B.get()
            mm(P, b3[:64, :CB], fw3[:, d, :], h2[:], True, True, r=["fw3", h2k], w=[k3])
            mm(P, b4[:64, :CB], e0[:], z[:], True, True, r=["e0", zk], w=[k4])
            wn, wnk = S_w.get(); hr, hrk = S_hr.get(); hb, hbk = S_hb.get(); sq, sqk = S_sq.get()
            act(P, wn[:], b4[:64, :CB], AF.Exp, r=[k4, "dneg"], w=[wnk], scale=dneg[:, 0:1])
            stt(P, hr[:], b3[:64, :CB], fb3[:, d:d + 1], wn[:], ALU.add, ALU.mult, r=[k3, "fb3", wnk], w=[hrk])
            act(P, sq[:], hr[:], AF.Square, r=[hrk], w=[sqk, "ssq"], accum_out=ssq[:, d * NCB + blk:d * NCB + blk + 1])
            cp(P, "dve", hb[:], hr[:], r=[hrk], w=[hbk])
            if d == 0:
                P.dma("sp", gs[:, cs], hb[:], (hbk, "st"), r=[hbk], w=["gs"])
            else:
                lo = 1 if blk == 0 else 0
                P.dma("sp", gs[:, L - 1 + CB * blk + lo: L - 1 + CB * blk + CB], hb[:, lo:CB], (hbk, "st"), r=[hbk], w=["gs"])
    rn = P.sb("rn", [64, 1]); rnb = P.sb("rnb", [64, 128]); rnrep = P.sb("rnrep", [128, 64])
    P.op("dve", lambda e: e.tensor_reduce(rn[:], ssq[:], AX.X, ALU.add), r=["ssq"], w=["rn"])
    act(P, rn[:], rn[:], AF.Sqrt, r=["rn"], w=["rn"], bias=1e-6)
    recip(P, rn[:], rn[:], r=["rn"], w=["rn"])
    ts(P, "dve", rnb[:], ones[:64, :], rn[:, 0:1], None, ALU.mult, None, r=["ones", "rn"], w=["rnb"])
    bb, bbk = B.get()
    mm(P, bb[:, :64], rnb[:], ident[:64, :64], True, True, r=["rnb", "ident"], w=[bbk])
    cp(P, "act", rnrep[:], bb[:, :64], r=[bbk], w=["rnrep"])

    SCB = min(2048, L)
    S_xi = Stage(P, "c_xi", [96, SCB + 2], F32, 2); S_uc = Stage(P, "c_uc", [96, SCB], F32, 2)
    for b in range(2):
        for blk in range(L // SCB):
            c0 = blk * SCB
            xi, xik = S_xi.get(); uc, uck = S_uc.get()
            lo = max(c0 - 1, 0); hi = min(c0 + SCB + 1, L)
            if c0 == 0:
                P.op("pool", lambda e, o_=xi[:, 0:1]: e.memset(o_, 0.0), w=[xik])
            if c0 + SCB == L:
                P.op("pool", lambda e, o_=xi[:, SCB + 1:SCB + 2]: e.memset(o_, 0.0), w=[xik])
            P.dma("sp", xi[:, lo - (c0 - 1): hi - (c0 - 1)], hyd[:, b, lo:hi], (xik, "ld"), w=[xik])
            act(P, uc[:], xi[:, 1:SCB + 1], AF.Identity, r=[xik, "cw"], w=[uck], bias=cw[:, 3:4], scale=cw[:, 1:2])
            stt(P, uc[:], xi[:, 0:SCB], cw[:, 0:1], uc[:], ALU.mult, ALU.add, r=[xik, "cw", uck], w=[uck])
            stt(P, uc[:], xi[:, 2:SCB + 2], cw[:, 2:3], uc[:], ALU.mult, ALU.add, r=[xik, "cw", uck], w=[uck])
            P.dma("sp", ucd[:, b, c0:c0 + SCB], uc[:], (uck, "st"), r=[uck], w=["ucd"])

    Fv = P.sb("Fv", [NB, 32, 2, 128]); Fx1 = P.sb("Fx1", [NB, 32, 2, 128]); Fx2 = P.sb("Fx2", [NB, 32, 2, 128])
    for t, k, r0 in ((Fv, "Fv", 0), (Fx1, "Fx1", 32), (Fx2, "Fx2", 64)):
        for b in range(2):
            P.dma("sp", t[:, :, b, :], ucd[r0:r0 + 32, b, :].rearrange("r (I i) -> I r i", i=128), (k, "ld"), r=["ucd"], w=[k])

    NE = 2 * NB - 1
    EQ = 64
    NQ = (NE + EQ - 1) // EQ
    WQ = min(EQ, NE) * 128
    S_W = Stage(P, "Wq", [128, WQ + 1], BF16, 2)
    S_U = Stage(P, "Uc", [128, NB, 2], BF16, 2)
    S_Y = Stage(P, "Yp", [128, NB, 2], F32, 2)
    S_A = Stage(P, "ga", [NB, 2, 128], F32, 2)
    q0 = (NB - 1) // EQ
    qorder = [q0] + [q for q in range(NQ) if q != q0]
    cnt = 0
    for o in range(2):
        Fin, Fink = (Fv, "Fv") if o == 0 else (Fx1, "Fx1")
        Fg, Fgk = (Fx1, "Fx1") if o == 0 else (Fx2, "Fx2")
        for cl in range(32):
            row = o * 32 + cl
            ub, ubk = B.get()
            for b in range(2):
                P.op("pe", lambda e, o_=ub[:, b * NB:(b + 1) * NB], i_=Fin[:, cl, b, :], idn=ident[:NB, :NB]:
                     e.transpose(o_, i_, idn), r=[Fink, "ident"], w=[ubk])
            U, Uk = S_U.get()
            cp(P, "act", U[:].rearrange("p n b -> p b n"), ub[:, :2 * NB].rearrange("p (b n) -> p b n", b=2), r=[ubk], w=[Uk])
            U2 = U[:].rearrange("p n b -> p (n b)")
            yb = ybanks[cnt % 2]; ybk = f"psy{cnt % 2}"
            cnt += 1
            first = True
            nmm = NE
            done = 0
            for q in qorder:
                E0 = q * EQ
                nE = min(EQ, NE - E0)
                W, Wk = S_W.get()
                wlen = nE * 128
                src = bass.AP(tensor=gs.tensor, offset=row * 2 * L + E0 * 128, ap=[[1, 128], [1, wlen]])
                P.dma("sp", W[:, :wlen], src, (Wk, "ld"), r=["gs"], w=[Wk])
                Es = list(range(E0, E0 + nE))
                if first:
                    Es = [NB - 1] + [E for E in Es if E != NB - 1]
                for E in Es:
                    d = NB - 1 - E
                    J0 = max(0, -d); J1 = min(NB - 1, NB - 1 - d) + 1
                    done += 1
                    P.op("pe", lambda e, o_=yb[:, 2 * (J0 + d):2 * (J1 + d)], l_=W[:, (E - E0) * 128:(E - E0) * 128 + 128],
                         r_=U2[:, 2 * J0:2 * J1], st=first, sp=(done == nmm):
                         e.matmul(o_, l_, r_, start=st, stop=sp, skip_group_check=True), r=[Wk, Uk], w=[ybk])
                    first = False
            Y, Yk = S_Y.get()
            cp(P, "act", Y[:].rearrange("p n b -> p b n"), yb[:, :2 * NB].rearrange("p (n b) -> p b n", b=2), r=[ybk], w=[Yk])
            fb_, fbk = B.get()
            for b in range(2):
                mm(P, fb_[:NB, b * 128:(b + 1) * 128], Y[:].rearrange("p n b -> p b n")[:, b, :], anti[:], True, True,
                   r=[Yk, "anti"], w=[fbk])
            a, ak = S_A.get()
            ts(P, "dve", a[:].rearrange("p b n -> p (b n)"), fb_[:NB, :256], rnrep[:NB, row:row + 1], None, ALU.mult, None,
               r=[fbk, "rnrep"], w=[ak])
            stt(P, a[:], Fin[:, cl, :, :], skip[:NB, o, cl:cl + 1], a[:], ALU.mult, ALU.add, r=[Fink, "skip", ak], w=[ak])
            tt(P, "dve", Fg[:, cl, :, :], a[:], Fg[:, cl, :, :], ALU.mult, r=[ak, Fgk], w=[Fgk])
    for b in range(2):
        P.dma("sp", o_yb[:, b, :].rearrange("r (I i) -> I r i", i=128), Fx2[:, :, b, :], ("Fx2", "st"), r=["Fx2"], is_out=True)
    return None


def _hy_consts(L):
    C = _consts()
    key = ("hyz", L)
    if key not in C:
        pos = np.arange(L, dtype=np.float32)
        t = pos / np.float32(L - 1)
        freqs = np.linspace(1e-4, 15, 16, dtype=np.float32)
        ang = (np.float32(2.0 * np.pi) * pos / np.float32(L)).astype(np.float32)[:, None] * freqs
        z = np.concatenate([t[:, None], np.cos(ang), -np.sin(ang)], -1).astype(np.float32)
        ZT = np.ascontiguousarray(z.T)
        C[key] = (ZT, np.ascontiguousarray(ZT[:, ::-1]))
        lo = np.log(np.float32(1e-2)) / np.float32(1.5); hi = np.log(np.float32(1e-2)) / np.float32(0.3)
        C["hydelta"] = np.abs(np.linspace(lo, hi, 256, dtype=np.float32))
        e0 = np.zeros((33, 64), np.float32); e0[0] = 1.0
        C["e0"] = e0
        C["anti"] = np.ascontiguousarray(np.eye(128, dtype=np.float32)[::-1])
    return C[key]


def hyena_inputs(hy_b, inp, l, L):
    C = _consts()
    ZT, ZTr = _hy_consts(L)
    w3 = inp["hy_f_w3"][l]; b3 = inp["hy_f_b3"][l]
    maps = []
    for c in range(NCORES):
        ch = np.concatenate([part * 256 + 32 * c + np.arange(32) for part in range(3)])
        hy = np.ascontiguousarray(np.stack([hy_b[b][ch] for b in range(BATCH)], 1), dtype=np.float32)
        cw = np.ascontiguousarray(np.concatenate([inp["hy_conv_w"][l][:, ch].T, inp["hy_conv_b"][l][ch][:, None]], 1))
        fw3c = np.zeros((64, 2, 64), np.float32); fb3c = np.zeros((64, 2), np.float32)
        for o in range(2):
            for d in range(2):
                cols = (o * 2 + d) * 256 + 32 * c + np.arange(32)
                fw3c[:, d, o * 32:(o + 1) * 32] = w3[:, cols]
                fb3c[o * 32:(o + 1) * 32, d] = b3[cols]
        dl = C["hydelta"][32 * c:32 * c + 32]
        dneg = np.ascontiguousarray(-np.concatenate([dl, dl])[:, None])
        skip = np.ascontiguousarray(np.broadcast_to(inp["hy_skip"][l][:, 32 * c:32 * c + 32][None], (128, 2, 32)), dtype=np.float32)
        maps.append({
            "hy": hy, "cw": cw, "ZT": ZT, "ZTr": ZTr, "fw1": inp["hy_f_w1"][l],
            "fv1": np.ascontiguousarray(np.stack([inp["hy_f_freq1"][l], inp["hy_f_b1"][l]], 1)),
            "fw2": inp["hy_f_w2"][l],
            "fv2": np.ascontiguousarray(np.stack([inp["hy_f_freq2"][l], inp["hy_f_b2"][l]], 1)),
            "fw3c": fw3c, "fb3c": fb3c, "dneg": dneg, "e0": C["e0"], "skip": skip,
            "ident": C["ident"], "anti": C["anti"], "ones": C["ones"],
        })
    return maps


NK = CTX + SEQ
NKB = NK // 128
MLA_SCALE = float(96 ** -0.5)


def build_mla(need_ctx):
    P = Prog()
    emit_mla(P, need_ctx)
    return P.finish()


def emit_mla(P, need_ctx):
    S2 = [P.ps(f"pss{i}", [128, 1024]) for i in range(3)]
    s2i = [0]

    def s2get():
        i = s2i[0] % 3
        s2i[0] += 1
        return S2[i], f"pss{i}"

    class _B:
        @staticmethod
        def get():
            return s2get()

    B = _B()
    accs = [P.ps("psa0", [128, 512]), P.ps("psa1", [128, 512])]
    QAd = P.din("QA", [2, 97, SEQ], BF16); KAd = P.din("KA", [2, 97, NK], BF16)
    VAd = P.din("VA", [128, 2, NKB, 65], BF16)
    w1d = P.din("w1", [97, 1]); onesd = P.din("ones", [128, 128])
    o_att = P.dout("o_att", [2, 64, SEQ])
    if need_ctx:
        QCd = P.din("QC", [2, 97, CTX], BF16)
        o_attc = P.dout("o_attc", [2, 64, CTX])
    w1 = P.sb("w1", [97, 1]); ones = P.sb("ones", [128, 128])
    P.dma("sp", w1[:], w1d, ("w1", "ld"), w=["w1"])
    P.dma("sp", ones[:], onesd, ("ones", "ld"), w=["ones"])
    KA = [P.sb(f"KA{h}", [97, NK], BF16) for h in range(2)]
    VA = [P.sb(f"VA{h}", [128, NKB, 65], BF16) for h in range(2)]
    kmx = P.sb("kmx", [1, 40]); nk = P.sb("nk", [1, 2])
    S_sq = Stage(P, "m_sq", [97, 512], F32, 2); S_Q = Stage(P, "m_Q", [97, 512], BF16, 2)
    S_qn = Stage(P, "m_qn", [1, 512], F32, 2); S_P = Stage(P, "m_P", [128, 2, 512], BF16, 4)
    S_o = Stage(P, "m_o", [65, 512], F32, 2); S_y = Stage(P, "m_y", [64, 512], F32, 2)
    nacc = [0]

    def qblock(h, Qsrc, q0, N, nkb, dst):
        Q, Qk = S_Q.get()
        P.dma("sp", Q[:, :N], Qsrc[h, :, q0:q0 + N], (Qk, "ld"), w=[Qk])
        sq, sqk = S_sq.get()
        act(P, sq[:, :N], Q[:, :N], AF.Square, r=[Qk], w=[sqk])
        b1, k1 = B.get()
        mm(P, b1[:1, :N], w1[:], sq[:, :N], True, True, r=["w1", sqk], w=[k1])
        qn, qnk = S_qn.get()
        act(P, qn[:, :N], b1[:1, :N], AF.Sqrt, r=[k1], w=[qnk])
        ts(P, "dve", Q[0:1, :N], qn[:, :N], nk[0:1, h:h + 1], None, ALU.mult, None, r=[qnk, "nk"], w=[Qk])
        acc = accs[nacc[0] % 2]; acck = f"psa{nacc[0] % 2}"
        nacc[0] += 1
        npair = nkb // 2
        pend = {}

        def issue_qk(pr):
            t, tk = s2get()
            for j in range(2):
                kb = 2 * pr + j
                mm(P, t[:, 512 * j:512 * j + N], KA[h][:, kb * 128:(kb + 1) * 128], Q[:, :N], True, True,
                   r=[f"KA{h}", Qk], w=[tk])
            pend[pr] = (t, tk)

        for pr in range(min(2, npair)):
            issue_qk(pr)
        for pr in range(npair):
            t, tk = pend.pop(pr)
            Pt, Pk = S_P.get()
            act(P, Pt[:, :, :N], t[:].rearrange("p (j n) -> p j n", j=2)[:, :, :N], AF.Exp, r=[tk], w=[Pk],
                scale=MLA_SCALE)
            if pr + 2 < npair:
                issue_qk(pr + 2)
            for j in range(2):
                kb = 2 * pr + j
                mm(P, acc[:65, :N], VA[h][:, kb, :], Pt[:, j, :N], kb == 0, kb == nkb - 1, r=[f"VA{h}", Pk], w=[acck])
        o, ok = S_o.get()
        cp(P, "dve", o[:, :N], acc[:65, :N], r=[acck], w=[ok])
        recip(P, o[64:65, :N], o[64:65, :N], r=[ok], w=[ok])
        b2, k2 = B.get()
        mm(P, b2[:64, :N], ones[64:65, 0:64], o[64:65, :N], True, True, r=["ones", ok], w=[k2])
        y, yk = S_y.get()
        tt(P, "dve", y[:, :N], o[0:64, :N], b2[:64, :N], ALU.mult, r=[ok, k2], w=[yk])
        P.dma("sp", dst[h, :, q0:q0 + N], y[:, :N], (yk, "st"), r=[yk], is_out=True)

    for h in range(2):
        for i in range(4):
            c0 = i * (NK // 4)
            P.dma("sp", KA[h][:, c0:c0 + NK // 4], KAd[h, :, c0:c0 + NK // 4], (f"KA{h}", "ld"), w=[f"KA{h}"])
        P.dma("sp", VA[h][:], VAd[:, h, :, :], (f"VA{h}", "ld"), w=[f"VA{h}"])
        nblk = (NK + 511) // 512
        for blk in range(nblk):
            c0 = blk * 512
            N = min(512, NK - c0)
            sq, sqk = S_sq.get()
            act(P, sq[:, :N], KA[h][:, c0:c0 + N], AF.Square, r=[f"KA{h}"], w=[sqk])
            b1, k1 = B.get()
            mm(P, b1[:1, :N], w1[:], sq[:, :N], True, True, r=["w1", sqk], w=[k1])
            P.op("dve", lambda e, o_=kmx[:, blk:blk + 1], i_=b1[:1, :N]: e.tensor_reduce(o_, i_, AX.X, ALU.max),
                 r=[k1], w=["kmx"])
        P.op("dve", lambda e, o_=nk[:, h:h + 1], i_=kmx[:, :nblk]: e.tensor_reduce(o_, i_, AX.X, ALU.max),
             r=["kmx"], w=["nk"])
        act(P, nk[:, h:h + 1], nk[:, h:h + 1], AF.Sqrt, r=["nk"], w=["nk"])
        ts(P, "dve", nk[:, h:h + 1], nk[:, h:h + 1], -1.0, None, ALU.mult, None, r=["nk"], w=["nk"])
        for qb in range(SEQ // 512):
            qblock(h, QAd, qb * 512, 512, NKB, o_att)
        if need_ctx:
            qblock(h, QCd, 0, CTX, CTX // 128, o_attc)
    return None


def mla_inputs(s1res, need_ctx):
    C = _consts()
    if "w1" not in C:
        w1 = np.ones((97, 1), np.float32); w1[0] = 0.0
        C["w1"] = w1
    bf = ml_dtypes.bfloat16
    maps = []
    QTl, QTc, Knl, Knc, krl, krc, Vl, Vc = [], [], [], [], [], [], [], []
    for b in range(BATCH):
        cores = [s1res[4 * b + j] for j in range(4)]
        QTl.append(np.concatenate([r["o_QT"][:, :, :RL] for r in cores], -1)); QTc.append(np.concatenate([r["o_QT"][:, :, RL:] for r in cores], -1))
        Knl.append(np.concatenate([r["o_KnT"][:, :, :RL] for r in cores], -1)); Knc.append(np.concatenate([r["o_KnT"][:, :, RL:] for r in cores], -1))
        krl.append(np.concatenate([r["o_kr"][:, :RL] for r in cores], -1)); krc.append(np.concatenate([r["o_kr"][:, RL:] for r in cores], -1))
        Vl.append(np.concatenate([r["o_V"][:RL] for r in cores], 0)); Vc.append(np.concatenate([r["o_V"][RL:] for r in cores], 0))
    for c in range(NCORES):
        b, p = c // 4, c % 4
        QA = np.zeros((2, 97, SEQ), bf); KA = np.ones((2, 97, NK), bf); VA = np.ones((128, 2, NKB, 65), bf)
        QC = np.zeros((2, 97, CTX), bf)
        for i in range(2):
            h = 2 * p + i
            QA[i, 1:] = QTl[b][h]; QC[i, 1:] = QTc[b][h]
            KA[i, 1:65, :CTX] = Knc[b][h]; KA[i, 1:65, CTX:] = Knl[b][h]
            KA[i, 65:, :CTX] = krc[b]; KA[i, 65:, CTX:] = krl[b]
            V = np.concatenate([Vc[b][:, 64 * h:64 * h + 64], Vl[b][:, 64 * h:64 * h + 64]], 0)
            VA[:, i, :, :64] = V.reshape(NKB, 128, 64).transpose(1, 0, 2)
        m = {"QA": QA, "KA": KA, "VA": VA, "w1": C["w1"], "ones": C["ones"]}
        if need_ctx:
            m["QC"] = QC
        maps.append(m)
    return maps


ALPHA = float((2.0 * 2) ** 0.25)
FB = 512


def emit_ln(P, B, ones, u, uk, N, gcol, bcol, out_fn, tmps):
    usq, usqk, st, stk = tmps
    for k in range(8):
        act(P, usq[:, k, :N], u[:, k, :N], AF.Square, r=[uk], w=[f"{usqk}_{k}"])
    b1, k1 = B.get(); b2, k2 = B.get()
    for k in range(8):
        mm(P, b1[:, :N], ones[:], u[:, k, :N], k == 0, k == 7, r=["ones", uk], w=[k1])
    for k in range(8):
        mm(P, b2[:, :N], ones[:], usq[:, k, :N], k == 0, k == 7, r=["ones", f"{usqk}_{k}"], w=[k2])
    ts(P, "dve", st[:, 0, :N], b1[:, :N], 1.0 / D, None, ALU.mult, None, r=[k1], w=[stk])
    tt(P, "dve", st[:, 2, :N], st[:, 0, :N], st[:, 0, :N], ALU.mult, r=[stk], w=[stk])
    stt(P, st[:, 1, :N], b2[:, :N], 1.0 / D, st[:, 2, :N], ALU.mult, ALU.subtract, r=[k2, stk], w=[stk])
    act(P, st[:, 1, :N], st[:, 1, :N], AF.Sqrt, r=[stk], w=[stk], bias=1e-5)
    recip(P, st[:, 1, :N], st[:, 1, :N], r=[stk], w=[stk])
    tt(P, "dve", st[:, 2, :N], st[:, 0, :N], st[:, 1, :N], ALU.mult, r=[stk], w=[stk])
    for k in range(8):
        kk = f"{usqk}_{k}"
        e1 = "pool" if k % 2 else "dve"
        tt(P, e1, usq[:, k, :N], u[:, k, :N], st[:, 1, :N], ALU.mult, r=[uk, stk], w=[kk])
        tt(P, e1, usq[:, k, :N], usq[:, k, :N], st[:, 2, :N], ALU.subtract, r=[kk, stk], w=[kk])
        out_fn(k, usq[:, k, :N], kk)


def build_s4(kind, need_ctx):
    P = Prog()
    emit_s4(P, kind, need_ctx)
    return P.finish()


def emit_s4(P, kind, need_ctx):
    NE_ = 1 if kind == "dense" else 8
    FF = 2816 if kind == "dense" else 3584
    R4 = RR if need_ctx else RL
    B = Banks(P)
    xd = P.din("x", [R4, D]); yad = P.din("ya", [256, R4]); ybd = P.din("yb", [256, R4]); ycd = P.din("yc", [512, R4])
    modd = P.din("mod", [128, 48, 2]); identd = P.din("ident", [128, 128]); onesd = P.din("ones", [128, 128])
    hygd = P.din("hyg", [128, 2]); mlagd = P.din("mlag", [128, 4]); woutd = P.din("wout", [D, D])
    lngd = P.din("lng", [128, 2, 8]); lnbd = P.din("lnb", [128, 2, 8])
    w1d = P.din("w1", [NE_, D, FF]); w3d = P.din("w3", [NE_, D, FF]); w2d = P.din("w2", [NE_, FF, D])
    if kind == "moe":
        wrd = P.din("wr", [D, 8]); seld = P.din("sel", [8, 8, 128])
    o_xT = P.dout("o_xT", [D, R4])
    x1d = P.dscr("x1_scratch", [D, R4], F32)
    hmd = P.dscr("hm_scratch", [D, R4], BF16)
    gtd = P.dscr("gt_scratch", [8, R4], F32)

    def ld(name, shape, src, dt=F32, eng="sp"):
        t = P.sb(name, shape, dt)
        P.dma(eng, t[:], src, (name, "ld"), w=[name])
        return t

    ident = ld("ident", [128, 128], identd); ones = ld("ones", [128, 128], onesd)
    modT = ld("modT", [128, 48, 2], modd); lng = ld("lng", [128, 2, 8], lngd); lnb = ld("lnb", [128, 2, 8], lnbd)
    sc2p = P.sb("sc2p", [128, 8, 2])
    ts(P, "dve", sc2p[:], modT[:, 32:40, :], 1.0, None, ALU.add, None, r=["modT"], w=["sc2p"])
    groups = [(512 * i, 512, 0) for i in range(RL // 512)] + ([(RL, RC, 1)] if need_ctx else [])

    P.push_scope()
    hyg = ld("hyg", [128, 2], hygd); mlag = ld("mlag", [128, 4], mlagd)
    wout = ld("wout", [128, 8, D], woutd.rearrange("(k p) n -> p k n", p=128), BF16, "pool")
    if kind == "moe":
        wr = ld("wr", [128, 8, 8], wrd.rearrange("(k p) n -> p k n", p=128))
    xt_slots = [P.sb(f"xt{i}", [128, D]) for i in range(2)]
    S_xa = Stage(P, "xa", [128, 8, 512], F32, 1); S_yi = Stage(P, "yi", [128, 8, 512], F32, 1)
    S_sq = Stage(P, "ysq", [128, 6, 512], F32, 1); S_rs = Stage(P, "yrs", [128, 2, 512], F32, 1)
    S_yb = Stage(P, "ybf", [128, 8, 512], BF16, 2); S_u = Stage(P, "u1", [128, 8, 512], F32, 1)
    S_usq = Stage(P, "usq", [128, 8, 512], F32, 1); S_st = Stage(P, "lst", [128, 3, 512], F32, 1)
    S_x1 = Stage(P, "x1", [128, 8, 512], F32, 1); S_hm = Stage(P, "hm", [128, 8, 512], F32, 1)
    S_hb = Stage(P, "hmb", [128, 8, 512], BF16, 2)
    S_lg = Stage(P, "lg", [128, 8], F32, 2); S_e1 = Stage(P, "e1", [128, 8], F32, 2); S_e2 = Stage(P, "e2", [128, 8], F32, 2)
    S_m = Stage(P, "mx", [128, 4], F32, 2); S_gt = Stage(P, "gT", [8, 512], F32, 2)
    for gi, (row0, N, msel) in enumerate(groups):
        cs = slice(row0, row0 + N)
        xa, xak = S_xa.get()

        def evac(k, pap, bk, col0, rows, xa=xa, xak=xak):
            act(P, xa[:, k, col0:col0 + rows], pap, AF.Copy, r=[bk], w=[xak], scale=ALPHA)

        emit_load_xT(P, B, xd, row0, N, ident, xt_slots, gi, evac)
        yi, yik = S_yi.get()
        P.dma("sp", yi[:, 0:2, :N], yad.rearrange("(k p) n -> p k n", p=128)[:, :, cs], (yik, "ld"), w=[yik])
        P.dma("sp", yi[:, 2:4, :N], ybd.rearrange("(k p) n -> p k n", p=128)[:, :, cs], (yik, "ld"), w=[yik])
        P.dma("sp", yi[:, 4:8, :N], ycd.rearrange("(k p) n -> p k n", p=128)[:, :, cs], (yik, "ld"), w=[yik])
        sq, sqk = S_sq.get(); rs, rsk = S_rs.get(); ybf, ybk = S_yb.get()
        for k in range(2, 8):
            act(P, sq[:, k - 2, :N], yi[:, k, :N], AF.Square, r=[yik], w=[sqk])
        for j, (ks, nf) in enumerate((((0, 1), 256.0), ((2, 3, 4, 5), 512.0))):
            bank, bk = B.get()
            for i, k in enumerate(ks):
                mm(P, bank[:, :N], ones[:], sq[:, k, :N], i == 0, i == len(ks) - 1, r=["ones", sqk], w=[bk])
            act(P, rs[:, j, :N], bank[:, :N], AF.Sqrt, r=[bk], w=[rsk], bias=1e-6, scale=1.0 / nf)
            recip(P, rs[:, j, :N], rs[:, j, :N], r=[rsk], w=[rsk])
        for k in range(2):
            cp(P, "pool", ybf[:, k, :N], yi[:, k, :N], r=[yik], w=[ybk])
        for k in range(2, 8):
            gs_ = hyg[:, k - 2:k - 1] if k < 4 else mlag[:, k - 4:k - 3]
            stt(P, ybf[:, k, :N], yi[:, k, :N], gs_, rs[:, 0 if k < 4 else 1, :N], ALU.mult, ALU.mult,
                r=[yik, rsk, "hyg", "mlag"], w=[ybk])
        u, uk = S_u.get()
        for dc in range(8):
            bank, bk = B.get()
            for k in range(8):
                mm(P, bank[:, :N], wout[:, k, dc * 128:(dc + 1) * 128], ybf[:, k, :N], k == 0, k == 7, r=["wout", ybk], w=[bk])
            stt(P, u[:, dc, :N], bank[:, :N], modT[:, 16 + dc, msel:msel + 1], xa[:, dc, :N], ALU.mult, ALU.add,
                r=[bk, "modT", xak], w=[uk])
        usq, usqk = S_usq.get(); st, stk = S_st.get(); x1, x1k = S_x1.get(); hm, hmk = S_hm.get(); hb, hbk = S_hb.get()

        def out1(k, xn, xnk, x1=x1, x1k=x1k, hm=hm, hmk=hmk, hb=hb, hbk=hbk, N=N, msel=msel):
            act(P, x1[:, k, :N], xn, AF.Identity, r=[xnk, "lng", "lnb"], w=[x1k], bias=lnb[:, 0, k:k + 1], scale=lng[:, 0, k:k + 1])
            act(P, hm[:, k, :N], x1[:, k, :N], AF.Identity, r=[x1k, "sc2p", "modT"], w=[hmk],
                bias=modT[:, 24 + k, msel:msel + 1], scale=sc2p[:, k, msel:msel + 1])
            cp(P, "pool", hb[:, k, :N], hm[:, k, :N], r=[hmk], w=[hbk])

        emit_ln(P, B, ones, u, uk, N, None, None, out1, (usq, usqk, st, stk))
        P.dma("sp", x1d.rearrange("(k p) n -> p k n", p=128)[:, :, cs], x1[:, :, :N], (x1k, "st"), r=[x1k], w=["x1d"])
        P.dma("sp", hmd.rearrange("(k p) n -> p k n", p=128)[:, :, cs], hb[:, :, :N], (hbk, "st"), r=[hbk], w=["hmd"])
        if kind == "moe":
            gT, gTk = S_gt.get()
            gb, gbk = B.get()
            for t in range((N + 127) // 128):
                rows = min(128, N - 128 * t)
                bank, bk = B.get()
                for k in range(8):
                    mm(P, bank[:rows, 0:8], hm[:, k, 128 * t:128 * t + rows], wr[:, k, :], k == 0, k == 7, r=[hmk, "wr"], w=[bk])
                lg, lgk = S_lg.get(); e1, e1k = S_e1.get(); e2, e2k = S_e2.get(); mx, mxk = S_m.get()
                cp(P, "dve", lg[:rows, :], bank[:rows, 0:8], r=[bk], w=[lgk])
                P.op("dve", lambda e, o_=mx[:rows, 0:1], i_=lg[:rows, :]: e.tensor_reduce(o_, i_, AX.X, ALU.max), r=[lgk], w=[mxk])
                ts(P, "dve", e1[:rows, :], lg[:rows, :], mx[:rows, 0:1], None, ALU.is_equal, None, r=[lgk, mxk], w=[e1k])
                stt(P, e2[:rows, :], e1[:rows, :], -1e30, lg[:rows, :], ALU.mult, ALU.add, r=[e1k, lgk], w=[e2k])
                P.op("dve", lambda e, o_=mx[:rows, 1:2], i_=e2[:rows, :]: e.tensor_reduce(o_, i_, AX.X, ALU.max), r=[e2k], w=[mxk])
                ts(P, "dve", e2[:rows, :], e2[:rows, :], mx[:rows, 1:2], None, ALU.is_equal, None, r=[e2k, mxk], w=[e2k])
                tt(P, "dve", mx[:rows, 2:3], mx[:rows, 0:1], mx[:rows, 1:2], ALU.subtract, r=[mxk], w=[mxk])
                act(P, mx[:rows, 2:3], mx[:rows, 2:3], AF.Sigmoid, r=[mxk], w=[mxk])
                ts(P, "dve", mx[:rows, 3:4], mx[:rows, 2:3], -1.0, 1.0, ALU.mult, ALU.add, r=[mxk], w=[mxk])
                ts(P, "dve", e1[:rows, :], e1[:rows, :], mx[:rows, 2:3], None, ALU.mult, None, r=[e1k, mxk], w=[e1k])
                stt(P, e1[:rows, :], e2[:rows, :], mx[:rows, 3:4], e1[:rows, :], ALU.mult, ALU.add, r=[e2k, mxk, e1k], w=[e1k])
                P.op("pe", lambda e, o_=gb[:8, 128 * t:128 * t + rows], i_=e1[:rows, :], idn=ident[:rows, :rows]:
                     e.transpose(o_, i_, idn), r=[e1k, "ident"], w=[gbk])
            cp(P, "act", gT[:, :N], gb[:8, :N], r=[gbk], w=[gTk])
            P.dma("sp", gtd[:, cs], gT[:, :N], (gTk, "st"), r=[gTk], w=["gtd"])
    P.pop_scope()

    spans = [(0, 2048), (2048, 2048)] + ([(RL, RC)] if need_ctx else [])
    hmS = P.sb("hmS", [128, 8, 2048], BF16); acc = P.sb("accS", [128, 8, 2048], F32)
    if kind == "moe":
        sel = ld("sel", [8, 8, 128], seld)
        gts = P.sb("gts", [8, 2048]); Gb = P.sb("Gb", [128, 4, 512], F32)
    S_w1 = Stage(P, "fw1", [128, 8, FB], BF16, 2); S_w3 = Stage(P, "fw3", [128, 8, FB], BF16, 2)
    S_w2 = Stage(P, "fw2", [128, 4, D], BF16, 2)
    S_s = Stage(P, "fs", [128, 512], F32, 2); S_t = Stage(P, "ft", [128, 512], F32, 2)
    S_g = Stage(P, "fg", [128, 4, 512], BF16, 2)
    S_x1b = Stage(P, "x1b", [128, 8, 128], F32, 1)
    S_usq2 = Stage(P, "usq2", [128, 8, 128], F32, 1); S_st2 = Stage(P, "lst2", [128, 3, 128], F32, 1)
    S_out = Stage(P, "xo", [128, 8, 128], F32, 1)
    nfb = [(f0, min(FB, FF - f0)) for f0 in range(0, FF, FB)]
    for (t0, TN) in spans:
        msel = 1 if t0 >= RL else 0
        P.dma("sp", hmS[:, :, :TN], hmd.rearrange("(k p) n -> p k n", p=128)[:, :, t0:t0 + TN], ("hmS", "ld"), r=["hmd"], w=["hmS"])
        if kind == "moe":
            P.dma("sp", gts[:, :TN], gtd[:, t0:t0 + TN], ("gts", "ld"), r=["gtd"], w=["gts"])
        sub = [(g0, min(512, TN - g0)) for g0 in range(0, TN, 512)]
        pendB = []

        def phase_b(item):
            (w2t, w2k, nfc, g, gk, g0, N, first) = item
            for dc in range(8):
                bank, bk = B.get()
                for fc in range(nfc):
                    mm(P, bank[:, :N], w2t[:, fc, dc * 128:(dc + 1) * 128], g[:, fc, :N], fc == 0, fc == nfc - 1, r=[w2k, gk], w=[bk])
                if first:
                    cp(P, "dve", acc[:, dc, g0:g0 + N], bank[:, :N], r=[bk], w=["accS"])
                else:
                    tt(P, "dve", acc[:, dc, g0:g0 + N], acc[:, dc, g0:g0 + N], bank[:, :N], ALU.add, r=[bk, "accS"], w=["accS"])

        first_acc = True
        for e_ in range(NE_):
            if kind == "moe":
                for gi, (g0, N) in enumerate(sub):
                    bank, bk = B.get()
                    mm(P, bank[:, :N], sel[:, e_, :], gts[:, g0:g0 + N], True, True, r=["sel", "gts"], w=[bk])
                    cp(P, "act", Gb[:, gi, :N], bank[:, :N], r=[bk], w=["Gb"])
            for (f0, fn) in nfb:
                nfc = fn // 128
                w1t, w1k = S_w1.get(); w3t, w3k = S_w3.get(); w2t, w2k = S_w2.get()
                P.dma("pool", w1t[:, :, :fn], w1d[e_].rearrange("(k p) n -> p k n", p=128)[:, :, f0:f0 + fn], (w1k, "ld"), w=[w1k])
                P.dma("pool", w3t[:, :, :fn], w3d[e_].rearrange("(k p) n -> p k n", p=128)[:, :, f0:f0 + fn], (w3k, "ld"), w=[w3k])
                P.dma("pool", w2t[:, :nfc, :], w2d[e_, f0:f0 + fn, :].rearrange("(k p) n -> p k n", p=128), (w2k, "ld"), w=[w2k])
                for gi, (g0, N) in enumerate(sub):
                    g, gk = S_g.get()
                    for fc in range(nfc):
                        b1, k1 = B.get(); b3, k3 = B.get()
                        for k in range(8):
                            mm(P, b1[:, :N], w1t[:, k, fc * 128:(fc + 1) * 128], hmS[:, k, g0:g0 + N], k == 0, k == 7, r=[w1k, "hmS"], w=[k1])
                        for k in range(8):
                            mm(P, b3[:, :N], w3t[:, k, fc * 128:(fc + 1) * 128], hmS[:, k, g0:g0 + N], k == 0, k == 7, r=[w3k, "hmS"], w=[k3])
                        s_, sk = S_s.get()
                        act(P, s_[:, :N], b1[:, :N], AF.Silu, r=[k1], w=[sk])
                        if kind == "moe":
                            t_, tk = S_t.get()
                            tt(P, "dve", t_[:, :N], s_[:, :N], b3[:, :N], ALU.mult, r=[sk, k3], w=[tk])
                            tt(P, "pool", g[:, fc, :N], t_[:, :N], Gb[:, gi, :N], ALU.mult, r=[tk, "Gb"], w=[gk])
                        else:
                            tt(P, "dve", g[:, fc, :N], s_[:, :N], b3[:, :N], ALU.mult, r=[sk, k3], w=[gk])
                    if pendB:
                        phase_b(pendB.pop())
                    pendB.append((w2t, w2k, nfc, g, gk, g0, N, first_acc))
                first_acc = False
        if pendB:
            phase_b(pendB.pop())
        for (g0, N) in [(g0, min(128, TN - g0)) for g0 in range(0, TN, 128)]:
            cs = slice(t0 + g0, t0 + g0 + N)
            x1b, x1bk = S_x1b.get(); u2, u2k = x1b, x1bk
            P.dma("sp", x1b[:, :, :N], x1d.rearrange("(k p) n -> p k n", p=128)[:, :, cs], (x1bk, "ld"), r=["x1d"], w=[x1bk])
            for k in range(8):
                ts(P, "pool", x1b[:, k, :N], x1b[:, k, :N], ALPHA, None, ALU.mult, None, r=[x1bk], w=[x1bk])
                stt(P, u2[:, k, :N], acc[:, k, g0:g0 + N], modT[:, 40 + k, msel:msel + 1], x1b[:, k, :N], ALU.mult, ALU.add,
                    r=["accS", "modT", x1bk], w=[u2k])
            usq, usqk = S_usq2.get(); st, stk = S_st2.get(); xo, xok = S_out.get()

            def out2(k, xn, xnk, xo=xo, xok=xok, N=N):
                act(P, xo[:, k, :N], xn, AF.Identity, r=[xnk, "lng", "lnb"], w=[xok], bias=lnb[:, 1, k:k + 1], scale=lng[:, 1, k:k + 1])

            emit_ln(P, B, ones, u2, u2k, N, None, None, out2, (usq, usqk, st, stk))
            P.dma("sp", o_xT.rearrange("(k p) n -> p k n", p=128)[:, :, cs], xo[:, :, :N], (xok, "st"), r=[xok], is_out=True)
    return o_xT


def s4_inputs(l, inp, x_cur, xc_cur, s1res, glares, hyres, hycres, mlares, need_ctx):
    C = _consts()
    if "sel" not in C:
        sel = np.zeros((8, 8, 128), np.float32)
        for e in range(8):
            sel[e, e, :] = 1.0
        C["sel"] = sel
    R4 = RR if need_ctx else RL
    ya_l = [np.concatenate([glares[4 * b + h]["o_ya"][:, CTX:CTX + SEQ] for h in range(4)], 0) for b in range(BATCH)]
    ya_c = [np.concatenate([glares[4 * b + h]["o_ya"][:, CTX + SEQ:] for h in range(4)], 0) for b in range(BATCH)]
    yb_l = [np.concatenate([hyres[c]["o_yb"][:, b, :] for c in range(NCORES)], 0) for b in range(BATCH)]
    yc_l = [np.concatenate([mlares[4 * b + p]["o_att"].reshape(128, SEQ) for p in range(4)], 0) for b in range(BATCH)]
    if need_ctx:
        yb_c = [np.concatenate([hycres[c]["o_yb"][:, b, :] for c in range(NCORES)], 0) for b in range(BATCH)]
        yc_c = [np.concatenate([mlares[4 * b + p]["o_attc"].reshape(128, CTX) for p in range(4)], 0) for b in range(BATCH)]
    xcf = xc_cur.reshape(BATCH * CTX, D)
    dense = (l % 2 == 0)
    i = l // 2
    maps = []
    for c in range(NCORES):
        b, j = c // 4, c % 4
        ls = slice(RL * j, RL * (j + 1))
        cs = slice(RC * j, RC * (j + 1))
        def cat(lat, ctx):
            if need_ctx:
                return np.ascontiguousarray(np.concatenate([lat[b][:, ls], ctx[b][:, cs]], 1), dtype=np.float32)
            return np.ascontiguousarray(lat[b][:, ls], dtype=np.float32)
        xr = x_cur[b, ls]
        if need_ctx:
            xr = np.concatenate([xr, xcf[RC * c:RC * (c + 1)]], 0)
        m = {
            "x": np.ascontiguousarray(xr, dtype=np.float32), "ya": cat(ya_l, ya_c),
            "yb": cat(yb_l, yb_c if need_ctx else None), "yc": cat(yc_l, yc_c if need_ctx else None),
            "mod": s1res[c]["o_mod"], "ident": C["ident"], "ones": C["ones"],
            "hyg": _fm(inp["hy_norm_g"][l]), "mlag": _fm(inp["mla_norm_g"][l]), "wout": inp["w_out"][l],
            "lng": np.ascontiguousarray(inp["ln_g"][l].reshape(2, 8, 128).transpose(2, 0, 1)),
            "lnb": np.ascontiguousarray(inp["ln_b"][l].reshape(2, 8, 128).transpose(2, 0, 1)),
        }
        if dense:
            m["w1"] = inp["ffn_w1"][i][None]; m["w3"] = inp["ffn_w3"][i][None]; m["w2"] = inp["ffn_w2"][i][None]
        else:
            m["w1"] = inp["moe_w1"][i]; m["w3"] = inp["moe_w3"][i]; m["w2"] = inp["moe_w2"][i]
            m["wr"] = inp["moe_router"][i]; m["sel"] = C["sel"]
        maps.append(m)
    return maps


_PROGS = {}


def _prog(name, fn, *args):
    key = (name,) + args
    if key not in _PROGS:
        _PROGS[key] = fn(*args)
    return _PROGS[key]


def _run(nc, maps):
    return run_bass_kernel_spmd(nc, maps, core_ids=list(range(NCORES))).results


def _scoped(P, pfx, fn, *args):
    P.push_scope()
    P.pfx = pfx
    r = fn(P, *args)
    P.pop_scope()
    P.pfx = ""
    return r


def build_mix(need_ctx):
    P = Prog()
    _scoped(P, "g_", emit_gla)
    _scoped(P, "h_", emit_hyena, SEQ)
    if need_ctx:
        _scoped(P, "c_", emit_hyena, CTX)
    _scoped(P, "m_", emit_mla, need_ctx)
    return P.finish()


def build_s4s1(kind, need_ctx):
    P = Prog()
    xT = _scoped(P, "a_", emit_s4, kind, need_ctx)
    _scoped(P, "b_", emit_s1, xT)
    return P.finish()


def _pref(maps, pfx):
    return [{pfx + k: v for k, v in m.items()} for m in maps]


def _unpref(res, pfx):
    return [{k[len(pfx):]: v for k, v in r.items() if k.startswith(pfx)} for r in res]


def _merge(*lists):
    out = []
    for parts in zip(*lists):
        d = {}
        for p in parts:
            d.update(p)
        out.append(d)
    return out


def kernel(**inputs):
    inp = {k: np.asarray(v) for k, v in inputs.items()}
    x = np.ascontiguousarray(inp["x"], dtype=np.float32)
    xc = np.ascontiguousarray(inp["ctx"], dtype=np.float32)
    s1res = _run(_prog("s1", build_s1), s1_inputs(0, inp, x, xc))
    for l in range(2):
        need_ctx = l < 1
        hl, hc = _gather_fm(s1res, "o_hy")
        parts = [_pref(gla_inputs(s1res, inp, l), "g_"), _pref(hyena_inputs(hl, inp, l, SEQ), "h_")]
        if need_ctx:
            parts.append(_pref(hyena_inputs(hc, inp, l, CTX), "c_"))
        parts.append(_pref(mla_inputs(s1res, need_ctx), "m_"))
        mres = _run(_prog("mix", build_mix, need_ctx), _merge(*parts))
        glares = _unpref(mres, "g_"); hyres = _unpref(mres, "h_"); mlares = _unpref(mres, "m_")
        hycres = _unpref(mres, "c_") if need_ctx else None
        s4m = _pref(s4_inputs(l, inp, x, xc, s1res, glares, hyres, hycres, mlares, need_ctx), "a_")
        if l == 0:
            s1m = s1_inputs(1, inp, x, xc)
            for m in s1m:
                m.pop("x")
            res = _run(_prog("s4s1", build_s4s1, "dense", True), _merge(s4m, _pref(s1m, "b_")))
            s1res = _unpref(res, "b_")
            s4res = _unpref(res, "a_")
            xn = np.empty_like(x); xcn = np.empty((BATCH * CTX, D), np.float32)
            for c in range(NCORES):
                b, j = c // 4, c % 4
                o = s4res[c]["o_xT"]
                xn[b, RL * j:RL * (j + 1)] = o[:, :RL].T
                xcn[RC * c:RC * (c + 1)] = o[:, RL:].T
            x, xc = xn, xcn.reshape(BATCH, CTX, D)
        else:
            s4res = _unpref(_run(_prog("s4", lambda: _build_pref_s4()), s4m), "a_")
            out = np.empty_like(x)
            for c in range(NCORES):
                b, j = c // 4, c % 4
                out[b, RL * j:RL * (j + 1)] = s4res[c]["o_xT"].T
            return out


def _build_pref_s4():
    P = Prog()
    _scoped(P, "a_", emit_s4, "moe", False)
    return P.finish()
```
